# Optimizing a Trainium2 kernel written in Bass

```python
import jax, jax.numpy as jnp
from jax import lax
import numpy as np

D_MODEL = 2048
BATCH = 4
SEQ = 4096
DEPTH = 2

N_EVEN = (DEPTH + 1) // 2
N_ODD = DEPTH // 2
EPS = 1e-6

CONV_DIM = 1024
CONV_WIDTH = 3
MLA_HEADS = 8
Q_LORA = 512
KV_LORA = 256
QK_NOPE = 128
QK_ROPE = 64
V_HEAD = 128
ROPE_THETA = 10000.0
Q_BLOCK = 128
IN_COLS = 3 * CONV_DIM + Q_LORA + KV_LORA + QK_ROPE
MIX_OUT = CONV_DIM + MLA_HEADS * V_HEAD
POOL_WINDOWS = (2, 4, 8, 16)
POOL_GROUPS = 4
POOL_DIM = D_MODEL // POOL_GROUPS
D_FF_DENSE = 5632
N_EXPERTS = 8
TOP_K = 2
D_FF_EXPERT = 2816

kernel_name = "hybrid_conv_mla_pool_moe_block"


def rmsnorm(x, g):
    xf = x.astype(jnp.float32)
    y = xf * lax.rsqrt(jnp.mean(xf * xf, axis=-1, keepdims=True) + EPS)
    return (y * g.astype(jnp.float32)).astype(x.dtype)


def swiglu(h, w_gate, w_up, w_down):
    return (jax.nn.silu(h @ w_gate) * (h @ w_up)) @ w_down


def causal_short_conv(u, w):
    S = u.shape[1]
    up = jnp.pad(u, ((0, 0), (CONV_WIDTH - 1, 0), (0, 0)))
    return sum(w[j] * up[:, j:j + S] for j in range(CONV_WIDTH))


def rope(t, cos, sin):
    half = t.shape[-1] // 2
    tf = t.astype(jnp.float32)
    t1, t2 = tf[..., :half], tf[..., half:]
    return jnp.concatenate([t1 * cos - t2 * sin, t1 * sin + t2 * cos], axis=-1).astype(t.dtype)


def causal_block_attention(q_nope, q_rope, k_nope, k_rope, v):
    B, S, H, _ = q_nope.shape
    nb = S // Q_BLOCK
    scale = (QK_NOPE + QK_ROPE) ** -0.5
    key_pos = jnp.arange(S)

    def to_blocks(t):
        return jnp.moveaxis(t.reshape(B, nb, Q_BLOCK, *t.shape[2:]), 1, 0)

    def one_block(args):
        qn, qr, start = args
        s = (jnp.einsum('bqhd,bkhd->bhqk', qn, k_nope, preferred_element_type=jnp.float32)
             + jnp.einsum('bqhd,bkd->bhqk', qr, k_rope, preferred_element_type=jnp.float32))
        q_pos = start + jnp.arange(Q_BLOCK)
        mask = key_pos[None, :] <= q_pos[:, None]
        s = jnp.where(mask, s * scale, -jnp.inf)
        p = jax.nn.softmax(s, axis=-1).astype(v.dtype)
        return jnp.einsum('bhqk,bkhd->bqhd', p, v)

    starts = jnp.arange(nb) * Q_BLOCK
    out = lax.map(one_block, (to_blocks(q_nope), to_blocks(q_rope), starts))
    return jnp.moveaxis(out, 0, 1).reshape(B, S, H * V_HEAD)


def conv_mla_mixer(h, cos, sin, w_in, conv_w, q_norm, w_uq, kv_norm, w_ukv, w_out):
    B, S, _ = h.shape
    proj = h @ w_in
    o = np.cumsum([0, CONV_DIM, CONV_DIM, CONV_DIM, Q_LORA, KV_LORA, QK_ROPE])
    x_in, gate_b, gate_c, c_q, c_kv, k_r = [proj[..., o[i]:o[i + 1]] for i in range(6)]
    y_a = gate_b * causal_short_conv(gate_c * x_in, conv_w)
    q = (rmsnorm(c_q, q_norm) @ w_uq).reshape(B, S, MLA_HEADS, QK_NOPE + QK_ROPE)
    kv = (rmsnorm(c_kv, kv_norm) @ w_ukv).reshape(B, S, MLA_HEADS, QK_NOPE + V_HEAD)
    q_nope, q_rope = q[..., :QK_NOPE], rope(q[..., QK_NOPE:], cos[:, None, :], sin[:, None, :])
    k_nope, v = kv[..., :QK_NOPE], kv[..., QK_NOPE:]
    k_rope = rope(k_r, cos, sin)
    y_b = causal_block_attention(q_nope, q_rope, k_nope, k_rope, v)
    return jnp.concatenate([y_a, y_b], axis=-1) @ w_out


def multiscale_pool_mixer(h, pool_w, pool_scale):
    B, S, D = h.shape
    hg = h.reshape(B, S, POOL_GROUPS, POOL_DIM).astype(jnp.float32)
    cz = jnp.concatenate([jnp.zeros((B, 1, POOL_GROUPS, POOL_DIM), jnp.float32),
                          jnp.cumsum(hg, axis=1)], axis=1)
    t = jnp.arange(S)
    means = []
    for g, w in enumerate(POOL_WINDOWS):
        c = cz[:, :, g]
        upper = c[:, 1:]
        lower = jnp.concatenate([jnp.zeros((B, w - 1, POOL_DIM), jnp.float32), c[:, :S - w + 1]], axis=1)
        count = jnp.minimum(t + 1, w).astype(jnp.float32)[None, :, None]
        means.append((upper - lower) / count)
    pooled = (jnp.stack(means, axis=2) - hg).astype(h.dtype)
    y = jnp.einsum('bsgc,gcd->bsgd', pooled, pool_w).reshape(B, S, D)
    return y * pool_scale


def moe_swiglu(h, router_w, w_gate, w_up, w_down):
    logits = jnp.einsum('bsd,de->bse', h, router_w, preferred_element_type=jnp.float32)
    top_v, top_i = lax.top_k(logits, TOP_K)
    top_p = jax.nn.softmax(top_v, axis=-1)
    gates = jnp.sum(jax.nn.one_hot(top_i, N_EXPERTS, dtype=jnp.float32) * top_p[..., None],
                    axis=-2).astype(h.dtype)
    y = jnp.zeros_like(h)
    for e in range(N_EXPERTS):
        y = y + gates[..., e:e + 1] * swiglu(h, w_gate[e], w_up[e], w_down[e])
    return y


def setup_inputs(seed: int = 0) -> dict:
    key = jax.random.key(seed)
    ks = jax.random.split(key, 24)

    def w(k, shape, fan_in):
        return jax.random.normal(k, shape, jnp.float32) * fan_in ** -0.5

    def gain(k, shape):
        return 1.0 + 0.02 * jax.random.normal(k, shape, jnp.float32)

    E, O = N_EVEN, N_ODD
    return {
        "x": jax.random.normal(ks[0], (BATCH, SEQ, D_MODEL), jnp.float32),
        "norm_mix0": gain(ks[1], (E, D_MODEL)),
        "w_in": w(ks[2], (E, D_MODEL, IN_COLS), D_MODEL),
        "conv_w": w(ks[3], (E, CONV_WIDTH, CONV_DIM), CONV_WIDTH),
        "q_norm": gain(ks[4], (E, Q_LORA)),
        "w_uq": w(ks[5], (E, Q_LORA, MLA_HEADS * (QK_NOPE + QK_ROPE)), Q_LORA),
        "kv_norm": gain(ks[6], (E, KV_LORA)),
        "w_ukv": w(ks[7], (E, KV_LORA, MLA_HEADS * (QK_NOPE + V_HEAD)), KV_LORA),
        "w_out": w(ks[8], (E, MIX_OUT, D_MODEL), MIX_OUT),
        "norm_ffn0": gain(ks[9], (E, D_MODEL)),
        "ffn_w_gate": w(ks[10], (E, D_MODEL, D_FF_DENSE), D_MODEL),
        "ffn_w_up": w(ks[11], (E, D_MODEL, D_FF_DENSE), D_MODEL),
        "ffn_w_down": w(ks[12], (E, D_FF_DENSE, D_MODEL), D_FF_DENSE),
        "norm_mix1": gain(ks[13], (O, D_MODEL)),
        "pool_w": w(ks[14], (O, POOL_GROUPS, POOL_DIM, POOL_DIM), POOL_DIM),
        "pool_scale": gain(ks[15], (O, D_MODEL)),
        "norm_ffn1": gain(ks[16], (O, D_MODEL)),
        "router_w": w(ks[17], (O, D_MODEL, N_EXPERTS), D_MODEL),
        "moe_w_gate": w(ks[18], (O, N_EXPERTS, D_MODEL, D_FF_EXPERT), D_MODEL),
        "moe_w_up": w(ks[19], (O, N_EXPERTS, D_MODEL, D_FF_EXPERT), D_MODEL),
        "moe_w_down": w(ks[20], (O, N_EXPERTS, D_FF_EXPERT, D_MODEL), D_FF_EXPERT),
        "final_norm": gain(ks[21], (D_MODEL,)),
    }


def reference(x, norm_mix0, w_in, conv_w, q_norm, w_uq, kv_norm, w_ukv, w_out,
              norm_ffn0, ffn_w_gate, ffn_w_up, ffn_w_down,
              norm_mix1, pool_w, pool_scale, norm_ffn1, router_w,
              moe_w_gate, moe_w_up, moe_w_down, final_norm):
    S = x.shape[1]
    pos = jnp.arange(S, dtype=jnp.float32)
    inv_freq = ROPE_THETA ** (-jnp.arange(0, QK_ROPE, 2, dtype=jnp.float32) / QK_ROPE)
    ang = pos[:, None] * inv_freq[None, :]
    cos, sin = jnp.cos(ang), jnp.sin(ang)

    for layer in range(DEPTH):
        i = layer // 2
        if layer % 2 == 0:
            h = rmsnorm(x, norm_mix0[i])
            x = x + conv_mla_mixer(h, cos, sin, w_in[i], conv_w[i], q_norm[i], w_uq[i],
                                   kv_norm[i], w_ukv[i], w_out[i])
            h = rmsnorm(x, norm_ffn0[i])
            x = x + swiglu(h, ffn_w_gate[i], ffn_w_up[i], ffn_w_down[i])
        else:
            h = rmsnorm(x, norm_mix1[i])
            x = x + multiscale_pool_mixer(h, pool_w[i], pool_scale[i])
            h = rmsnorm(x, norm_ffn1[i])
            x = x + moe_swiglu(h, router_w[i], moe_w_gate[i], moe_w_up[i], moe_w_down[i])
    return rmsnorm(x, final_norm)
```

```python
import math
import os
import numpy as np
import concourse.bass as bass
import concourse.mybir as mybir
from concourse.bass_utils import run_bass_kernel_spmd

F32 = mybir.dt.float32
BF16 = mybir.dt.bfloat16
AF = mybir.ActivationFunctionType
ALU = mybir.AluOpType
AX = mybir.AxisListType

D = 2048
TOK = 2048
TB = 512
NBLK = int(os.environ.get("KNBLK", "4"))
EPS = 1e-6
SCALE = 192.0 ** -0.5
FE = 2816
NF = FE // 128
NS = 3
SLOT = 8192


class Prog:
    def __init__(self, nc, dry=False):
        self.nc = nc
        self.dry = dry
        self.ops = {e: [] for e in ("pe", "act", "dve", "pool", "sp")}
        self.sems = {}
        self.cnt = {}
        self.waited = {}
        self.lastw = {}
        self.readers = {}

    def sem(self, key):
        if key not in self.sems:
            self.sems[key] = self.nc.alloc_semaphore("s_" + key.replace(":", "_"))
            self.cnt[key] = 0
        return self.sems[key]

    def _deps(self, eng, reads, writes, extra=()):
        deps = {}

        def add(kv):
            if kv[1] > deps.get(kv[0], 0):
                deps[kv[0]] = kv[1]

        for r in reads:
            if r in self.lastw:
                add(self.lastw[r])
        for w in writes:
            if w in self.lastw:
                add(self.lastw[w])
            for kv in self.readers.get(w, ()):
                add(kv)
        for kv in extra:
            add(kv)
        waits = []
        for k, v in deps.items():
            if k == eng and eng == "pe":
                continue
            if self.waited.get((eng, k), 0) >= v:
                continue
            self.waited[(eng, k)] = v
            waits.append((self.sems[k], v))
        return waits

    def _commit(self, kv, reads, writes):
        for r in reads:
            self.readers.setdefault(r, []).append(kv)
        for w in writes:
            self.lastw[w] = kv
            self.readers[w] = []

    def op(self, eng, fn, reads=(), writes=()):
        if self.dry:
            return None
        s = self.sem(eng)
        waits = self._deps(eng, reads, writes)
        self.cnt[eng] += 1
        kv = (eng, self.cnt[eng])
        self._commit(kv, reads, writes)

        def f(e, waits=waits, fn=fn, s=s):
            for (ws, wv) in waits:
                e.wait_ge(ws, wv)
            fn(e).then_inc(s, 1)

        self.ops[eng].append(f)
        return kv

    def dma(self, eng, semkey, fns, reads=(), writes=()):
        if self.dry:
            return None
        key = "d:" + semkey
        s = self.sem(key)
        extra = [(key, self.cnt[key])] if self.cnt[key] > 0 else []
        waits = self._deps(eng, reads, writes, extra)
        self.cnt[key] += 16 * len(fns)
        kv = (key, self.cnt[key])
        self._commit(kv, reads, writes)

        def f(e, waits=waits, fns=fns, s=s):
            for (ws, wv) in waits:
                e.wait_ge(ws, wv)
            for fn in fns:
                fn(e).then_inc(s, 16)

        self.ops[eng].append(f)
        return kv

    def barrier(self, engs=("pe", "act", "dve")):
        if self.dry:
            return
        for e in engs:
            waits = []
            for k in engs:
                if k == e or k not in self.cnt:
                    continue
                v = self.cnt[k]
                if self.waited.get((e, k), 0) >= v:
                    continue
                self.waited[(e, k)] = v
                waits.append((self.sems[k], v))
            if waits:
                self.ops[e].append(lambda en, waits=waits: [en.wait_ge(ws, wv) for (ws, wv) in waits])

    def wait_all(self, eng, kvs):
        if self.dry:
            return
        waits = []
        for k, v in kvs:
            if self.waited.get((eng, k), 0) >= v:
                continue
            self.waited[(eng, k)] = v
            waits.append((self.sems[k], v))
        self.ops[eng].append(lambda en, waits=waits: [en.wait_ge(ws, wv) for (ws, wv) in waits])

    def emit(self):
        with self.nc.Block() as block:
            @block.tensor
            def _(e):
                for f in self.ops["pe"]:
                    f(e)

            @block.scalar
            def _(e):
                for f in self.ops["act"]:
                    f(e)

            @block.vector
            def _(e):
                for f in self.ops["dve"]:
                    f(e)

            @block.gpsimd
            def _(e):
                for f in self.ops["pool"]:
                    f(e)

            @block.sync
            def _(e):
                for f in self.ops["sp"]:
                    f(e)


class WStream:
    def __init__(self, P, slots, sched=None):
        self.P = P
        self.slots = slots
        self.dry = sched is None
        self.sched = [] if sched is None else sched
        self.i = 0
        self.issued = 0

    def get(self, fns):
        if self.dry:
            self.sched.append(fns)
            return (len(self.sched) - 1) % NS
        i = self.i
        self.i += 1
        while self.issued < min(len(self.sched), i + NS):
            j = self.issued
            s = j % NS
            t = self.slots[s]
            self.P.dma("pool", f"ws{s}", [(lambda e, f=f, t=t: f(e, t)) for f in self.sched[j]], writes=[f"ws{s}"])
            self.issued += 1
        return i % NS


class Arena:
    def __init__(self, t, nbytes):
        self.t = t
        self.n = nbytes
        self.off = 0

    def reset(self):
        self.off = 0

    def get(self, shape, dtype):
        esz = 4 if dtype == F32 else 2
        nb = int(np.prod(shape[1:])) * esz
        nb_al = (nb + 63) // 64 * 64
        assert self.off + nb_al <= self.n, (self.off, nb_al, self.n)
        ap = self.t[0:shape[0], self.off // 4:(self.off + nb) // 4]
        if dtype != F32:
            ap = ap.bitcast(dtype)
        if len(shape) == 3:
            ap = ap.rearrange("p (a b) -> p a b", b=shape[2])
        self.off += nb_al
        return ap


ARENA_BYTES = 47 * 1024


def build(stop="y"):
    nc = bass.Bass("TRN2", target_bir_lowering=False)

    def din(name, shape):
        return nc.dram_tensor(name, list(shape), F32, kind="ExternalInput").ap()

    xo = din("xo", [TOK, D])
    xp = din("xp", [TOK, D])
    pbias_d = din("pbias", [128, 1])
    iota_d = din("iota", [64, TB])
    cf_d = din("cf", [64, 4])
    pfix_d = din("pfix", [128, 64])
    ident_d = din("ident", [128, 128])
    tri_d = din("tri", [128, 128])
    norm_mix0 = din("norm_mix0", [D])
    w_in = din("w_in", [D, 3904])
    conv_w = din("conv_w", [3, 1024])
    q_norm = din("q_norm", [512])
    w_uq = din("w_uq", [512, 1536])
    kv_norm = din("kv_norm", [256])
    w_ukv = din("w_ukv", [256, 2048])
    w_out = din("w_out", [D, D])
    norm_ffn0 = din("norm_ffn0", [D])
    ffn_w_gate = din("ffn_w_gate", [D, 5632])
    ffn_w_up = din("ffn_w_up", [D, 5632])
    ffn_w_down = din("ffn_w_down", [5632, D])
    norm_mix1 = din("norm_mix1", [D])
    pool_w = din("pool_w", [4, 512, 512])
    pool_scale = din("pool_scale", [D])
    norm_ffn1 = din("norm_ffn1", [D])
    router_w = din("router_w", [D, 8])
    moe_w_gate = din("moe_w_gate", [8, D, FE])
    moe_w_up = din("moe_w_up", [8, D, FE])
    moe_w_down = din("moe_w_down", [8, FE, D])
    final_norm = din("final_norm", [D])
    out = nc.dram_tensor("out", [TOK, D], F32, kind="ExternalOutput").ap()

    sb = nc.alloc_sbuf_tensor
    xacc = sb("xacc", [128, 4, D], F32)
    YT = sb("YT", [128, 16, TB], BF16)
    ckvnT = sb("ckvnT", [128, 2, 4096], BF16)
    krT = sb("krT", [64, 4096], BF16)
    cqnT = sb("cqnT", [128, 4, TB], BF16)
    slots = [sb(f"wslot{i}", [128, SLOT], BF16) for i in range(NS)]
    hb = sb("hb", [128, D], BF16)
    cs_sb = sb("cs_sb", [64, 2, TB], F32)
    psc = sb("psc", [128, 512], F32)
    fgbc = sb("fgbc", [128, D], F32)
    ot = sb("ot", [128, D], F32)
    iot = sb("iot", [64, TB], F32)
    cfs = sb("cfs", [64, 4], F32)
    gT = sb("gT", [128, 128], F32)
    identb = sb("identb", [128, 128], BF16)
    onesb = sb("onesb", [128, 128], BF16)
    trib = sb("trib", [128, 128], BF16)
    wkrrot = sb("wkrrot", [128, 16, 64], BF16)
    wqrot = sb("wqrot", [128, 4, 64], BF16)
    uh = sb("uh", [128, 8, 2], F32)
    h1halo = sb("h1halo", [128, 16, 16], BF16)
    pbias = sb("pbias_sb", [128, 1], F32)
    zbias = sb("zbias", [128, 1], F32)
    pfix = sb("pfix_sb", [128, 64], F32)
    st = sb("st", [128, 32], F32)
    junk = sb("junk", [128, 512], BF16)
    rt = sb("rt", [128, 64], F32)
    gates = sb("gates", [128, 4, 8], F32)
    arena_t = sb("arena", [128, ARENA_BYTES // 4], F32)
    AR = Arena(arena_t, ARENA_BYTES)

    psT = [nc.alloc_psum_tensor(f"psT{i}", [128, 8, 128], BF16) for i in range(2)]
    psA = [nc.alloc_psum_tensor(f"psA{i}", [128, 512], F32) for i in range(6)]

    def kcv(ap2d):
        return ap2d.rearrange("(kc p) n -> p kc n", p=128)

    def program(P, W):
        final_kvs = []
        AR.reset()
        gst = AR.get([128, 128], F32)
        identf = AR.get([128, 128], F32)
        trif = AR.get([128, 128], F32)
        P.dma("sp", "c6", [lambda e: e.dma_start(out=iot[:, :], in_=iota_d[:, :])], writes=["iot"])
        P.dma("sp", "c7", [lambda e: e.dma_start(out=cfs[:, :], in_=cf_d[:, :])], writes=["cfs"])

        P.dma("sp", "c0", [lambda e: e.dma_start(out=identf[:, :], in_=ident_d[:, :])], writes=["identf"])
        P.dma("sp", "c1", [lambda e: e.dma_start(out=trif[:, :], in_=tri_d[:, :])], writes=["trif"])
        P.dma("sp", "c2", [lambda e: e.dma_start(out=pbias[:, :], in_=pbias_d[:, :])], writes=["pbias"])
        P.dma("sp", "c3", [lambda e: e.dma_start(out=pfix[:, :], in_=pfix_d[:, :])], writes=["pfix"])
        P.dma("sp", "c4", [lambda e: e.dma_start(out=fgbc[:, :], in_=final_norm.rearrange("(o n) -> o n", o=1).to_broadcast([128, D]))], writes=["fgbc"])
        P.op("dve", lambda e: e.memset(gst[:, :], 0.0), writes=["gst"])
        P.op("dve", lambda e: e.memset(zbias[:, :], 0.0), writes=["zbias"])
        P.op("dve", lambda e: e.memset(uh[:, :, :], 0.0), writes=["uh"])
        P.op("dve", lambda e: e.memset(h1halo[:, :, :], 0.0), writes=["h1halo"])
        P.op("dve", lambda e: e.memset(onesb[:, :], 1.0), writes=["onesb"])
        vecs = [(norm_mix0, 0, 16), (norm_ffn0, 16, 16), (norm_mix1, 32, 16), (norm_ffn1, 48, 16), (q_norm, 64, 4), (kv_norm, 68, 2)]
        fns = [(lambda e, v=v, r=r, n=n: e.dma_start(out=gst[r:r + n, :], in_=v.rearrange("(c p) -> c p", p=128))) for (v, r, n) in vecs]
        fns.append(lambda e: e.dma_start(out=gst[70:94, :], in_=conv_w.rearrange("j (c p) -> (j c) p", p=128)))
        P.dma("sp", "c5", fns, writes=["gst"])
        P.op("dve", lambda e: e.tensor_copy(out=identb[:, :], in_=identf[:, :]), reads=["identf"], writes=["identb"])
        P.op("dve", lambda e: e.tensor_copy(out=trib[:, :], in_=trif[:, :]), reads=["trif"], writes=["trib"])
        P.op("pe", lambda e: e.transpose(out=psA[0][:, 0:128], in_=gst[:, :], identity=identf[:, :]), reads=["gst", "identf"], writes=["psA0"])
        P.op("dve", lambda e: e.tensor_copy(out=gT[:, :], in_=psA[0][:, 0:128]), reads=["psA0"], writes=["gT"])
        G_MIX0, G_FFN0, G_MIX1, G_FFN1, G_Q, G_KV, G_CONV = 0, 16, 32, 48, 64, 68, 70

        def load_x(src, row0, nt):
            for ti in range(nt):
                P.dma("sp", f"x{ti}", [lambda e, ti=ti: e.dma_start(out=xacc[:, ti, :], in_=src[row0 + ti * 128:row0 + (ti + 1) * 128, :])],
                      writes=[f"xacc{ti}"])

        def rstd_of(ti):
            c = 8 * ti
            for q in range(4):
                P.op("act", lambda e, q=q: e.activation(out=junk[:, :], in_=xacc[:, ti, q * 512:(q + 1) * 512], func=AF.Square, accum_out=st[:, c + q:c + q + 1]),
                     reads=[f"xacc{ti}"], writes=[f"st{ti}q{q}", "junk"])
            P.op("dve", lambda e: e.tensor_reduce(out=st[:, c + 4:c + 5], in_=st[:, c:c + 4], axis=AX.X, op=ALU.add),
                 reads=[f"st{ti}q{q}" for q in range(4)], writes=[f"st{ti}"])
            P.op("dve", lambda e: e.tensor_scalar(out=st[:, c + 5:c + 6], in0=st[:, c + 4:c + 5], scalar1=1.0 / D, scalar2=EPS, op0=ALU.mult, op1=ALU.add),
                 reads=[f"st{ti}"], writes=[f"st{ti}"])
            P.op("act", lambda e: e.activation(out=st[:, c + 6:c + 7], in_=st[:, c + 5:c + 6], func=AF.Sqrt), reads=[f"st{ti}"], writes=[f"st{ti}"])
            P.op("dve", lambda e: e.reciprocal(out=st[:, c + 7:c + 8], in_=st[:, c + 6:c + 7]), reads=[f"st{ti}"], writes=[f"st{ti}"])

        def norm_T(nt, grow, hT, col0=0):
            for ti in range(nt):
                rstd_of(ti)
                c = 8 * ti
                for half in range(2):
                    P.op("dve", lambda e, ti=ti, c=c, half=half: e.tensor_scalar(out=hb[:, half * 1024:(half + 1) * 1024], in0=xacc[:, ti, half * 1024:(half + 1) * 1024],
                                                                                 scalar1=st[:, c + 7:c + 8], scalar2=None, op0=ALU.mult),
                         reads=[f"xacc{ti}", f"st{ti}"], writes=[f"hb{half}"])
                    for g4 in (2 * half, 2 * half + 1):
                        pt = psT[g4 % 2]

                        def tr(e, g4=g4, pt=pt):
                            for j in range(4):
                                ins = e.transpose(out=pt[:, j, :], in_=hb[:, (g4 * 4 + j) * 128:(g4 * 4 + j + 1) * 128], identity=identb[:, :])
                            return ins
                        P.op("pe", tr, reads=[f"hb{half}", "identb"], writes=[f"psT{g4 % 2}"])
                        c0 = col0 + ti * 128
                        if g4 % 2 == 0:
                            P.op("dve", lambda e, g4=g4, pt=pt, c0=c0: e.tensor_tensor(
                                out=hT[:, g4 * 4:(g4 + 1) * 4, c0:c0 + 128], in0=pt[:, 0:4, :],
                                in1=gT[:, grow + g4 * 4:grow + g4 * 4 + 4].unsqueeze(2).to_broadcast([128, 4, 128]), op=ALU.mult),
                                reads=[f"psT{g4 % 2}", "gT"], writes=[f"hT{ti}"])
                        else:
                            def ev(e, g4=g4, pt=pt, c0=c0):
                                for j in range(4):
                                    kc = g4 * 4 + j
                                    ins = e.mul(out=hT[:, kc, c0:c0 + 128], in_=pt[:, j, :], mul=gT[:, grow + kc:grow + kc + 1])
                                return ins
                            P.op("act", ev, reads=[f"psT{g4 % 2}", "gT"], writes=[f"hT{ti}"])

        def hT_keys(nt):
            return [f"hT{ti}" for ti in range(nt)]

        def lat_norm(ps_list, nchunk, n, raw, sq, rk, gcol, dst_fn, dst_key, inv_dim):
            for j in range(nchunk):
                P.op("act", lambda e, j=j: e.activation(out=raw[:, j, 0:n], in_=ps_list[j][1][:, 0:n], func=AF.Copy), reads=[ps_list[j][0]], writes=[f"raw{j}"])
                P.op("act", lambda e, j=j: e.activation(out=sq[:, j, 0:n], in_=ps_list[j][1][:, 0:n], func=AF.Square), reads=[ps_list[j][0]], writes=[f"sq{j}"])

            def mm(e):
                for j in range(nchunk):
                    ins = e.matmul(psA[5][:, 0:n], lhsT=onesb[:, :], rhs=sq[:, j, 0:n], start=(j == 0), stop=(j == nchunk - 1))
                return ins
            P.op("pe", mm, reads=[f"sq{j}" for j in range(nchunk)] + ["onesb"], writes=["psA5"])
            P.op("dve", lambda e: e.tensor_scalar(out=rk[:, 0:n], in0=psA[5][:, 0:n], scalar1=inv_dim, scalar2=EPS, op0=ALU.mult, op1=ALU.add), reads=["psA5"], writes=["rk"])
            P.op("act", lambda e: e.activation(out=rk[:, 0:n], in_=rk[:, 0:n], func=AF.Sqrt), reads=["rk"], writes=["rk"])
            P.op("dve", lambda e: e.reciprocal(out=rk[:, 0:n], in_=rk[:, 0:n]), reads=["rk"], writes=["rk"])
            for j in range(nchunk):
                P.op("dve", lambda e, j=j: e.scalar_tensor_tensor(out=dst_fn(j), in0=raw[:, j, 0:n], scalar=gT[:, gcol + j:gcol + j + 1], in1=rk[:, 0:n], op0=ALU.mult, op1=ALU.mult),
                     reads=[f"raw{j}", "rk", "gT"], writes=[dst_key])

        MAGIC = 12582912.0
        C1 = 6.28125
        C2 = 2.0 * math.pi - 6.28125

        def rope_tables(n, p0, own, tmp1, tmp2, tmp3):
            ang, kk, rr = tmp1[0:64, 0:n], tmp2[0:64, 0:n], tmp3[0:64, 0:n]
            if own:
                P.op("dve", lambda e: e.tensor_scalar(out=ang, in0=iot[:, 0:n], scalar1=cfs[:, 1:2], scalar2=float(p0), op0=ALU.add, op1=ALU.add), reads=["iot", "cfs"], writes=["tmp1"])
            else:
                P.op("dve", lambda e: e.tensor_scalar(out=ang, in0=iot[:, 0:n], scalar1=float(p0), scalar2=None, op0=ALU.add), reads=["iot"], writes=["tmp1"])
            P.op("dve", lambda e: e.tensor_scalar(out=ang, in0=ang, scalar1=cfs[:, 0:1], scalar2=None, op0=ALU.mult), reads=["tmp1", "cfs"], writes=["tmp1"])
            P.op("dve", lambda e: e.tensor_scalar(out=kk, in0=ang, scalar1=1.0 / (2.0 * math.pi), scalar2=MAGIC, op0=ALU.mult, op1=ALU.add), reads=["tmp1"], writes=["tmp2"])
            P.op("dve", lambda e: e.tensor_scalar(out=kk, in0=kk, scalar1=MAGIC, scalar2=None, op0=ALU.subtract), reads=["tmp2"], writes=["tmp2"])
            P.op("dve", lambda e: e.scalar_tensor_tensor(out=rr, in0=kk, scalar=-C1, in1=ang, op0=ALU.mult, op1=ALU.add), reads=["tmp2", "tmp1"], writes=["tmp3"])
            P.op("dve", lambda e: e.scalar_tensor_tensor(out=rr, in0=kk, scalar=-C2, in1=rr, op0=ALU.mult, op1=ALU.add), reads=["tmp2", "tmp3"], writes=["tmp3"])
            P.op("dve", lambda e: e.tensor_scalar(out=rr, in0=rr, scalar1=-math.pi, scalar2=math.pi, op0=ALU.max, op1=ALU.min), reads=["tmp3"], writes=["tmp3"])
            P.op("act", lambda e: e.activation(out=cs_sb[:, 1, 0:n], in_=rr, func=AF.Sin), reads=["tmp3"], writes=["cs_sb"])
            P.op("dve", lambda e: e.scalar_tensor_tensor(out=kk, in0=rr, scalar=-1.0, in1=rr, op0=ALU.mult, op1=ALU.max), reads=["tmp3"], writes=["tmp2"])
            P.op("act", lambda e: e.activation(out=cs_sb[:, 0, 0:n], in_=kk, func=AF.Sin, bias=cfs[:, 3:4], scale=-1.0), reads=["tmp2", "cfs"], writes=["cs_sb"])

        def kv_latents(hT, nt, col0, own, p0, raw, sq, rk, tmp1, tmp2, tmp3):
            n = nt * 128
            s = W.get([lambda e, t: e.dma_start(out=t[:, 0:16 * 320].rearrange("p (k n) -> p k n", n=320), in_=kcv(w_in)[:, :, 3584:3904])])
            wv = slots[s][:, 0:16 * 320].rearrange("p (k n) -> p k n", n=320)
            wk = f"ws{s}"
            P.op("dve", lambda e: e.tensor_scalar(out=wkrrot[:, :, 0:32], in0=wv[:, :, 288:320], scalar1=-1.0, scalar2=None, op0=ALU.mult), reads=[wk], writes=["wkrrot"])
            P.op("dve", lambda e: e.tensor_copy(out=wkrrot[:, :, 32:64], in_=wv[:, :, 256:288]), reads=[wk], writes=["wkrrot"])
            rope_tables(n, p0, own, tmp1, tmp2, tmp3)
            for j in range(2):
                def mm(e, j=j):
                    for kc in range(16):
                        ins = e.matmul(psA[j][:, 0:n], lhsT=wv[:, kc, j * 128:(j + 1) * 128], rhs=hT[:, kc, 0:n], start=(kc == 0), stop=(kc == 15))
                    return ins
                P.op("pe", mm, reads=[wk] + hT_keys(nt), writes=[f"psA{j}"])
            lat_norm([("psA0", psA[0]), ("psA1", psA[1])], 2, n, raw, sq, rk, G_KV, lambda j: ckvnT[:, j, col0:col0 + n], "ckvnT", 1.0 / 256)

            def mmr(e):
                for kc in range(16):
                    ins = e.matmul(psA[2][0:64, 0:n], lhsT=wv[:, kc, 256:320], rhs=hT[:, kc, 0:n], start=(kc == 0), stop=(kc == 15))
                return ins
            P.op("pe", mmr, reads=[wk] + hT_keys(nt), writes=["psA2"])

            def mmr2(e):
                for kc in range(16):
                    ins = e.matmul(psA[3][0:64, 0:n], lhsT=wkrrot[:, kc, :], rhs=hT[:, kc, 0:n], start=(kc == 0), stop=(kc == 15))
                return ins
            P.op("pe", mmr2, reads=["wkrrot"] + hT_keys(nt), writes=["psA3"])
            P.op("dve", lambda e: e.tensor_tensor(out=tmp1[0:64, 0:n], in0=psA[2][0:64, 0:n], in1=cs_sb[:, 0, 0:n], op=ALU.mult), reads=["psA2", "cs_sb"], writes=["tmp1"])
            P.op("dve", lambda e: e.tensor_tensor(out=tmp2[0:64, 0:n], in0=psA[3][0:64, 0:n], in1=cs_sb[:, 1, 0:n], op=ALU.mult), reads=["psA3", "cs_sb"], writes=["tmp2"])
            P.op("dve", lambda e: e.tensor_tensor(out=krT[:, col0:col0 + n], in0=tmp1[0:64, 0:n], in1=tmp2[0:64, 0:n], op=ALU.add), reads=["tmp1", "tmp2"], writes=["krT"])

        def m1_alloc():
            AR.reset()
            hT = AR.get([128, 16, TB], BF16)
            raw = AR.get([128, 4, TB], F32)
            sq = AR.get([128, 4, TB], BF16)
            rk = AR.get([128, TB], F32)
            tmp1 = AR.get([128, TB], F32)
            tmp2 = AR.get([128, TB], F32)
            tmp3 = AR.get([128, TB], F32)
            return hT, raw, sq, rk, tmp1, tmp2, tmp3

        P.barrier()
        hT, raw, sq, rk, tmp1, tmp2, tmp3 = m1_alloc()
        for g in range(4):
            load_x(xp, g * 512, 4)
            norm_T(4, G_MIX0, hT)
            kv_latents(hT, 4, g * 512, False, g * 512, raw, sq, rk, tmp1, tmp2, tmp3)
        P.barrier()

        def expert(hT, aT, sg, nt, wg, wu, wd, gate_e):
            n = nt * 128
            for fp in range(NF // 2):
                s = W.get([lambda e, t, fp=fp: e.dma_start(out=t[:, 0:4096].rearrange("p (k n) -> p k n", n=256), in_=kcv(wg)[:, :, fp * 256:(fp + 1) * 256]),
                           lambda e, t, fp=fp: e.dma_start(out=t[:, 4096:8192].rearrange("p (k n) -> p k n", n=256), in_=kcv(wu)[:, :, fp * 256:(fp + 1) * 256])])
                wgv = slots[s][:, 0:4096].rearrange("p (k n) -> p k n", n=256)
                wuv = slots[s][:, 4096:8192].rearrange("p (k n) -> p k n", n=256)
                wk = f"ws{s}"
                for f2 in range(2):
                    f = fp * 2 + f2
                    bg = (f % 2) * 2
                    pg, pu = psA[bg], psA[bg + 1]

                    def mmg(e, f2=f2, pg=pg, wgv=wgv):
                        for kc in range(16):
                            ins = e.matmul(pg[:, 0:n], lhsT=wgv[:, kc, f2 * 128:(f2 + 1) * 128], rhs=hT[:, kc, 0:n], start=(kc == 0), stop=(kc == 15))
                        return ins

                    def mmu(e, f2=f2, pu=pu, wuv=wuv):
                        for kc in range(16):
                            ins = e.matmul(pu[:, 0:n], lhsT=wuv[:, kc, f2 * 128:(f2 + 1) * 128], rhs=hT[:, kc, 0:n], start=(kc == 0), stop=(kc == 15))
                        return ins
                    P.op("pe", mmg, reads=[wk] + hT_keys(nt), writes=[f"psA{bg}"])
                    P.op("pe", mmu, reads=[wk] + hT_keys(nt), writes=[f"psA{bg + 1}"])
                    sgb = sg[f % 2]
                    P.op("act", lambda e, pg=pg, sgb=sgb: e.activation(out=sgb[:, 0:n], in_=pg[:, 0:n], func=AF.Silu), reads=[f"psA{bg}"], writes=[f"sg{f % 2}"])
                    P.op("dve", lambda e, pu=pu, sgb=sgb, f=f: e.tensor_tensor(out=aT[:, f, 0:n], in0=pu[:, 0:n], in1=sgb[:, 0:n], op=ALU.mult),
                         reads=[f"psA{bg + 1}", f"sg{f % 2}"], writes=[f"aT{f}"])
            cnt = 0
            for c8 in range(8):
                s = W.get([lambda e, t, c8=c8: e.dma_start(out=t[:, 0:NF * 256].rearrange("p (k n) -> p k n", n=256), in_=wd.rearrange("(f p) n -> p f n", p=128)[:, :, c8 * 256:(c8 + 1) * 256])])
                wdv = slots[s][:, 0:NF * 256].rearrange("p (k n) -> p k n", n=256)
                wk = f"ws{s}"
                for ti in range(nt):
                    b = 4 + (cnt % 2)
                    cnt += 1
                    pd = psA[b]

                    def mmd(e, ti=ti, pd=pd, wdv=wdv):
                        for f in range(NF):
                            ins = e.matmul(pd[:, 0:256], lhsT=aT[:, f, ti * 128:(ti + 1) * 128], rhs=wdv[:, f, :], start=(f == 0), stop=(f == NF - 1))
                        return ins
                    P.op("pe", mmd, reads=[wk] + [f"aT{f}" for f in range(NF)], writes=[f"psA{b}"])
                    xs_ = xacc[:, ti, c8 * 256:(c8 + 1) * 256]
                    if gate_e is None:
                        P.op("dve", lambda e, pd=pd, xs_=xs_: e.tensor_tensor(out=xs_, in0=pd[:, 0:256], in1=xs_, op=ALU.add),
                             reads=[f"psA{b}", f"xacc{ti}"], writes=[f"xacc{ti}"])
                    else:
                        P.op("dve", lambda e, pd=pd, xs_=xs_, ti=ti: e.scalar_tensor_tensor(out=xs_, in0=pd[:, 0:256], scalar=gates[:, ti, gate_e:gate_e + 1], in1=xs_, op0=ALU.mult, op1=ALU.add),
                             reads=[f"psA{b}", f"xacc{ti}", "gates"], writes=[f"xacc{ti}"])

        def dump(row0, nt):
            for ti in range(nt):
                kv = P.dma("sp", f"o{ti}", [lambda e, ti=ti: e.dma_start(out=out[row0 + ti * 128:row0 + (ti + 1) * 128, :], in_=xacc[:, ti, :])], reads=[f"xacc{ti}"])
                final_kvs.append(kv)

        def do_block(blk):
            halo = blk < 0
            nt = 1 if halo else 4
            n = nt * 128
            q0 = 15 if halo else 16 + 4 * blk
            nk = q0 + nt
            hT, raw, sq, rk, tmp1, tmp2, tmp3 = m1_alloc()
            u = AR.get([128, TB + 2], F32)
            tb = AR.get([128, TB], F32)
            xs = AR.get([128, TB], F32)
            gbs = AR.get([128, TB], F32)
            if halo:
                load_x(xp, 1920, 1)
            else:
                load_x(xo, blk * TB, 4)
            norm_T(nt, G_MIX0, hT)
            for c in range(8):
                s = W.get([(lambda e, t, c=c, j=j: e.dma_start(out=t[:, 0:6144].rearrange("p (k j n) -> p k j n", j=3, n=128)[:, :, j, :],
                                                               in_=kcv(w_in)[:, :, j * 1024 + c * 128:j * 1024 + (c + 1) * 128])) for j in range(3)])
                wv = slots[s][:, 0:6144].rearrange("p (k j n) -> p k j n", j=3, n=128)
                wk = f"ws{s}"
                for j in range(3):
                    def mm(e, j=j, wv=wv):
                        for kc in range(16):
                            ins = e.matmul(psA[j][:, 0:n], lhsT=wv[:, kc, j, :], rhs=hT[:, kc, 0:n], start=(kc == 0), stop=(kc == 15))
                        return ins
                    P.op("pe", mm, reads=[wk] + hT_keys(nt), writes=[f"psA{j}"])
                P.op("act", lambda e: e.activation(out=xs[:, 0:n], in_=psA[0][:, 0:n], func=AF.Copy), reads=["psA0"], writes=["xs"])
                P.op("act", lambda e: e.activation(out=gbs[:, 0:n], in_=psA[1][:, 0:n], func=AF.Copy), reads=["psA1"], writes=["gbs"])
                P.op("dve", lambda e, c=c: e.tensor_copy(out=u[:, 0:2], in_=uh[:, c, :]), reads=["uh"], writes=["u"])
                P.op("dve", lambda e: e.tensor_tensor(out=u[:, 2:2 + n], in0=psA[2][:, 0:n], in1=xs[:, 0:n], op=ALU.mult), reads=["psA2", "xs"], writes=["u"])
                P.op("dve", lambda e, c=c: e.tensor_scalar(out=tb[:, 0:n], in0=u[:, 2:2 + n], scalar1=gT[:, G_CONV + 16 + c:G_CONV + 17 + c], scalar2=None, op0=ALU.mult),
                     reads=["u", "gT"], writes=["tb"])
                P.op("dve", lambda e, c=c: e.scalar_tensor_tensor(out=tb[:, 0:n], in0=u[:, 1:1 + n], scalar=gT[:, G_CONV + 8 + c:G_CONV + 9 + c], in1=tb[:, 0:n], op0=ALU.mult, op1=ALU.add),
                     reads=["u", "gT", "tb"], writes=["tb"])
                P.op("dve", lambda e, c=c: e.scalar_tensor_tensor(out=tb[:, 0:n], in0=u[:, 0:n], scalar=gT[:, G_CONV + c:G_CONV + 1 + c], in1=tb[:, 0:n], op0=ALU.mult, op1=ALU.add),
                     reads=["u", "gT", "tb"], writes=["tb"])
                P.op("dve", lambda e, c=c: e.tensor_copy(out=uh[:, c, :], in_=u[:, n:n + 2]), reads=["u"], writes=["uh"])
                P.op("dve", lambda e, c=c: e.tensor_tensor(out=YT[:, c, 0:n], in0=tb[:, 0:n], in1=gbs[:, 0:n], op=ALU.mult), reads=["tb", "gbs"], writes=[f"YT{c}"])
            s = W.get([lambda e, t: e.dma_start(out=t[:, :].rearrange("p (k n) -> p k n", n=512), in_=kcv(w_in)[:, :, 3072:3584])])
            wv = slots[s][:, :].rearrange("p (k n) -> p k n", n=512)
            wk = f"ws{s}"
            for j in range(4):
                def mm(e, j=j, wv=wv):
                    for kc in range(16):
                        ins = e.matmul(psA[j][:, 0:n], lhsT=wv[:, kc, j * 128:(j + 1) * 128], rhs=hT[:, kc, 0:n], start=(kc == 0), stop=(kc == 15))
                    return ins
                P.op("pe", mm, reads=[wk] + hT_keys(nt), writes=[f"psA{j}"])
            lat_norm([(f"psA{j}", psA[j]) for j in range(4)], 4, n, raw, sq, rk, G_Q, lambda j: cqnT[:, j, 0:n], "cqnT", 1.0 / 512)
            if not halo:
                kv_latents(hT, 4, 2048 + blk * TB, True, blk * TB, raw, sq, rk, tmp1, tmp2, tmp3)
            else:
                rope_tables(128, 1920, False, tmp1, tmp2, tmp3)
            P.barrier()

            AR.reset()
            HB = []
            for i in range(2):
                HB.append(dict(KnT=AR.get([128, 4096], BF16), Vh=AR.get([128, 32, 128], BF16), QnT=AR.get([128, TB], BF16), qrT=AR.get([64, TB], BF16)))
            PT = [AR.get([128, TB], BF16) for _ in range(3)]
            rs = AR.get([128, TB], F32)
            t1 = AR.get([128, TB], F32)
            t2 = AR.get([128, TB], F32)

            def gen(h):
                hs = h % 2
                KnT, Vh, QnT, qrT = HB[hs]["KnT"], HB[hs]["Vh"], HB[hs]["QnT"], HB[hs]["qrT"]
                s = W.get([lambda e, t, h=h: e.dma_start(out=t[:, 0:768].rearrange("p (k n) -> p k n", n=192), in_=kcv(w_uq)[:, :, h * 192:(h + 1) * 192]),
                           lambda e, t, h=h: e.dma_start(out=t[:, 1024:1536].rearrange("p (k n) -> p k n", n=256), in_=kcv(w_ukv)[:, :, h * 256:(h + 1) * 256])])
                wq = slots[s][:, 0:768].rearrange("p (k n) -> p k n", n=192)
                wkv = slots[s][:, 1024:1536].rearrange("p (k n) -> p k n", n=256)
                wk = f"ws{s}"
                P.op("dve", lambda e: e.tensor_scalar(out=wqrot[:, :, 0:32], in0=wq[:, :, 160:192], scalar1=-1.0, scalar2=None, op0=ALU.mult), reads=[wk], writes=["wqrot"])
                P.op("dve", lambda e: e.tensor_copy(out=wqrot[:, :, 32:64], in_=wq[:, :, 128:160]), reads=[wk], writes=["wqrot"])
                ngk = (nk * 128 + 511) // 512
                for kg in range(ngk):
                    c0 = kg * 512
                    cn = min(512, nk * 128 - c0)
                    b = 4 + (kg % 2)

                    def mmk(e, c0=c0, cn=cn, b=b):
                        for kc in range(2):
                            ins = e.matmul(psA[b][:, 0:cn], lhsT=wkv[:, kc, 0:128], rhs=ckvnT[:, kc, c0:c0 + cn], start=(kc == 0), stop=(kc == 1))
                        return ins
                    P.op("pe", mmk, reads=[wk, "ckvnT"], writes=[f"psA{b}"])
                    P.op("act", lambda e, c0=c0, cn=cn, b=b: e.activation(out=KnT[:, c0:c0 + cn], in_=psA[b][:, 0:cn], func=AF.Copy), reads=[f"psA{b}"], writes=[f"KnT{hs}"])
                for vg in range((nk + 3) // 4):
                    k0 = vg * 4
                    kn = min(4, nk - k0)
                    b = 4 + (vg % 2)

                    def mmv(e, k0=k0, kn=kn, b=b):
                        for j in range(kn):
                            for kc in range(2):
                                ins = e.matmul(psA[b][:, j * 128:(j + 1) * 128], lhsT=ckvnT[:, kc, (k0 + j) * 128:(k0 + j + 1) * 128], rhs=wkv[:, kc, 128:256], start=(kc == 0), stop=(kc == 1))
                        return ins
                    P.op("pe", mmv, reads=[wk, "ckvnT"], writes=[f"psA{b}"])
                    P.op("dve", lambda e, k0=k0, kn=kn, b=b: e.tensor_copy(out=Vh[:, k0:k0 + kn, :], in_=psA[b][:, 0:kn * 128].rearrange("p (a b) -> p a b", b=128)),
                         reads=[f"psA{b}"], writes=[f"Vh{hs}"])

                def mmq(e):
                    for kc in range(4):
                        ins = e.matmul(psA[4][:, 0:n], lhsT=wq[:, kc, 0:128], rhs=cqnT[:, kc, 0:n], start=(kc == 0), stop=(kc == 3))
                    return ins
                P.op("pe", mmq, reads=[wk, "cqnT"], writes=["psA4"])
                P.op("act", lambda e: e.mul(out=QnT[:, 0:n], in_=psA[4][:, 0:n], mul=SCALE), reads=["psA4"], writes=[f"QnT{hs}"])

                def mmqr(e):
                    for kc in range(4):
                        ins = e.matmul(psA[5][0:64, 0:n], lhsT=wq[:, kc, 128:192], rhs=cqnT[:, kc, 0:n], start=(kc == 0), stop=(kc == 3))
                    return ins
                P.op("pe", mmqr, reads=[wk, "cqnT"], writes=["psA5"])
                P.op("dve", lambda e: e.tensor_tensor(out=t1[0:64, 0:n], in0=psA[5][0:64, 0:n], in1=cs_sb[:, 0, 0:n], op=ALU.mult), reads=["psA5", "cs_sb"], writes=["t1"])

                def mmqr2(e):
                    for kc in range(4):
                        ins = e.matmul(psA[5][0:64, 0:n], lhsT=wqrot[:, kc, :], rhs=cqnT[:, kc, 0:n], start=(kc == 0), stop=(kc == 3))
                    return ins
                P.op("pe", mmqr2, reads=["wqrot", "cqnT"], writes=["psA5"])
                P.op("dve", lambda e: e.tensor_tensor(out=t2[0:64, 0:n], in0=psA[5][0:64, 0:n], in1=cs_sb[:, 1, 0:n], op=ALU.mult), reads=["psA5", "cs_sb"], writes=["t2"])
                P.op("dve", lambda e: e.tensor_tensor(out=t1[0:64, 0:n], in0=t1[0:64, 0:n], in1=t2[0:64, 0:n], op=ALU.add), reads=["t1", "t2"], writes=["t1"])
                P.op("dve", lambda e: e.tensor_scalar(out=qrT[:, 0:n], in0=t1[0:64, 0:n], scalar1=SCALE, scalar2=None, op0=ALU.mult), reads=["t1"], writes=[f"qrT{hs}"])

            def attn(h):
                hs = h % 2
                KnT, Vh, QnT, qrT = HB[hs]["KnT"], HB[hs]["Vh"], HB[hs]["QnT"], HB[hs]["qrT"]

                def c0_of(j):
                    return 0 if j < q0 else (j - q0) * 128

                def emit_S(j):
                    c0 = c0_of(j)
                    b = j % 2

                    def mms(e):
                        e.matmul(psA[b][:, c0:n], lhsT=KnT[:, j * 128:(j + 1) * 128], rhs=QnT[:, c0:n], start=True, stop=False)
                        return e.matmul(psA[b][:, c0:n], lhsT=krT[:, j * 128:(j + 1) * 128], rhs=qrT[:, c0:n], start=False, stop=True)
                    P.op("pe", mms, reads=[f"KnT{hs}", f"QnT{hs}", "krT", f"qrT{hs}"], writes=[f"psA{b}"])
                    pt = PT[j % 3]
                    bias = pbias if j < 16 else zbias
                    P.op("act", lambda e: e.activation(out=pt[:, c0:n], in_=psA[b][:, c0:n], func=AF.Exp, bias=bias[:, 0:1], scale=1.0),
                         reads=[f"psA{b}", "pbias", "zbias"], writes=[f"PT{j % 3}"])
                    if j >= q0:
                        P.op("dve", lambda e: e.tensor_tensor(out=pt[:, c0:c0 + 128], in0=pt[:, c0:c0 + 128], in1=trib[:, :], op=ALU.mult),
                             reads=[f"PT{j % 3}", "trib"], writes=[f"PT{j % 3}"])

                def emit_PV(j):
                    c0 = c0_of(j)
                    pt = PT[j % 3]
                    P.op("pe", lambda e: e.matmul(psA[2][:, c0:n], lhsT=Vh[:, j, :], rhs=pt[:, c0:n], start=(j == 0), stop=(j == nk - 1)),
                         reads=[f"Vh{hs}", f"PT{j % 3}"], writes=["psA2"])
                    P.op("pe", lambda e: e.matmul(psA[3][:, c0:n], lhsT=onesb[:, :], rhs=pt[:, c0:n], start=(j == 0), stop=(j == nk - 1)),
                         reads=["onesb", f"PT{j % 3}"], writes=["psA3"])

                emit_S(0)
                for j in range(nk):
                    if j + 1 < nk:
                        emit_S(j + 1)
                    emit_PV(j)
                P.op("dve", lambda e: e.tensor_scalar(out=rs[:, 0:n], in0=psA[3][:, 0:n], scalar1=1e-30, scalar2=None, op0=ALU.add), reads=["psA3"], writes=["rs"])
                P.op("dve", lambda e: e.reciprocal(out=rs[:, 0:n], in_=rs[:, 0:n]), reads=["rs"], writes=["rs"])
                P.op("dve", lambda e: e.tensor_tensor(out=YT[:, 8 + h, 0:n], in0=psA[2][:, 0:n], in1=rs[:, 0:n], op=ALU.mult), reads=["psA2", "rs"], writes=[f"YT{8 + h}"])

            gen(0)
            for h in range(8):
                if h + 1 < 8:
                    gen(h + 1)
                attn(h)
            cnt = 0
            for cb in range(4):
                s = W.get([lambda e, t, cb=cb: e.dma_start(out=t[:, :].rearrange("p (k n) -> p k n", n=512), in_=kcv(w_out)[:, :, cb * 512:(cb + 1) * 512])])
                wv = slots[s][:, :].rearrange("p (k n) -> p k n", n=512)
                wk = f"ws{s}"
                for ti in range(nt):
                    b = 4 + (cnt % 2)
                    cnt += 1

                    def mmo2(e, ti=ti, b=b, wv=wv):
                        for kc in range(16):
                            ins = e.matmul(psA[b][:, :], lhsT=YT[:, kc, ti * 128:(ti + 1) * 128], rhs=wv[:, kc, :], start=(kc == 0), stop=(kc == 15))
                        return ins
                    P.op("pe", mmo2, reads=[wk] + [f"YT{c}" for c in range(16)], writes=[f"psA{b}"])
                    xs_ = xacc[:, ti, cb * 512:(cb + 1) * 512]
                    P.op("dve", lambda e, b=b, xs_=xs_: e.tensor_tensor(out=xs_, in0=psA[b][:, :], in1=xs_, op=ALU.add), reads=[f"psA{b}", f"xacc{ti}"], writes=[f"xacc{ti}"])
            P.barrier()
            if stop == "x1":
                if not halo:
                    dump(blk * TB, 4)
                return

            AR.reset()
            hT = AR.get([128, 16, TB], BF16)
            aT = AR.get([128, NF, TB], BF16)
            sg = [AR.get([128, TB], F32) for _ in range(2)]
            norm_T(nt, G_FFN0, hT)
            for hf in range(2):
                expert(hT, aT, sg, nt, ffn_w_gate[:, hf * FE:(hf + 1) * FE], ffn_w_up[:, hf * FE:(hf + 1) * FE], ffn_w_down[hf * FE:(hf + 1) * FE, :], None)
            P.barrier()
            if stop == "x2":
                if not halo:
                    dump(blk * TB, 4)
                return

            AR.reset()
            hTp = AR.get([128, 16, 16 + TB], BF16)
            norm_T(nt, G_MIX1, hTp, col0=16)
            if halo:
                P.op("dve", lambda e: e.tensor_copy(out=h1halo[:, :, :], in_=hTp[:, :, 16 + 112:16 + 128]), reads=hT_keys(1), writes=["h1halo"])
                P.barrier()
                return
            pooledT = AR.get([128, 16, TB], BF16)
            sA = AR.get([128, 16 + TB], F32)
            sB = AR.get([128, 16 + TB], F32)
            ptmp = AR.get([128, 512], F32)
            P.op("dve", lambda e: e.tensor_copy(out=hTp[:, :, 0:16], in_=h1halo[:, :, :]), reads=["h1halo"], writes=["hThalo"])
            P.op("dve", lambda e: e.tensor_copy(out=h1halo[:, :, :], in_=hTp[:, :, n:n + 16]), reads=hT_keys(nt) + ["hThalo"], writes=["h1halo"])
            for g in range(4):
                wwin = 2 ** (g + 1)
                for kc in range(4):
                    ch = g * 4 + kc
                    src = hTp[:, ch, :]
                    sk = None
                    bufs = [sA, sB]
                    for li in range(g + 1):
                        sh = 2 ** li
                        lo = 2 * sh - 1
                        dst = bufs[li % 2]
                        dk = "sA" if li % 2 == 0 else "sB"
                        rd = (hT_keys(nt) + ["hThalo"]) if li == 0 else [sk]
                        P.op("dve", lambda e, src=src, dst=dst, lo=lo, sh=sh: e.tensor_tensor(out=dst[:, lo:16 + n], in0=src[:, lo:16 + n], in1=src[:, lo - sh:16 + n - sh], op=ALU.add),
                             reads=rd, writes=[dk])
                        src = dst
                        sk = dk
                    if blk == 0:
                        P.op("dve", lambda e, src=src, g=g: e.tensor_tensor(out=src[:, 16:32], in0=src[:, 16:32], in1=pfix[:, g * 16:(g + 1) * 16], op=ALU.mult),
                             reads=[sk, "pfix"], writes=[sk])
                    P.op("dve", lambda e, src=src, ch=ch, wwin=wwin: e.scalar_tensor_tensor(out=pooledT[:, ch, 0:n], in0=src[:, 16:16 + n], scalar=1.0 / wwin, in1=hTp[:, ch, 16:16 + n], op0=ALU.mult, op1=ALU.subtract),
                         reads=[sk] + hT_keys(nt), writes=[f"pT{ch}"])
                s = W.get([lambda e, t, g=g: e.dma_start(out=t[:, 0:2048].rearrange("p (k n) -> p k n", n=512), in_=pool_w[g].rearrange("(kc p) n -> p kc n", p=128))])
                wv = slots[s][:, 0:2048].rearrange("p (k n) -> p k n", n=512)
                wk = f"ws{s}"
                P.dma("sp", "psc", [lambda e, g=g: e.dma_start(out=psc[:, :], in_=pool_scale[g * 512:(g + 1) * 512].rearrange("(o n) -> o n", o=1).to_broadcast([128, 512]))], writes=["psc"])
                for ti in range(nt):
                    b = 4 + (ti % 2)

                    def mmp(e, ti=ti, b=b, wv=wv, g=g):
                        for kc in range(4):
                            ins = e.matmul(psA[b][:, :], lhsT=pooledT[:, g * 4 + kc, ti * 128:(ti + 1) * 128], rhs=wv[:, kc, :], start=(kc == 0), stop=(kc == 3))
                        return ins
                    P.op("pe", mmp, reads=[wk] + [f"pT{g * 4 + kc}" for kc in range(4)], writes=[f"psA{b}"])
                    xs_ = xacc[:, ti, g * 512:(g + 1) * 512]
                    P.op("dve", lambda e, b=b: e.tensor_tensor(out=ptmp[:, :], in0=psA[b][:, :], in1=psc[:, :], op=ALU.mult), reads=[f"psA{b}", "psc"], writes=["ptmp"])
                    P.op("dve", lambda e, xs_=xs_: e.tensor_tensor(out=xs_, in0=ptmp[:, :], in1=xs_, op=ALU.add), reads=["ptmp", f"xacc{ti}"], writes=[f"xacc{ti}"])
            P.barrier()
            if stop == "x3":
                dump(blk * TB, 4)
                return

            AR.reset()
            hT = AR.get([128, 16, TB], BF16)
            aT = AR.get([128, NF, TB], BF16)
            sg = [AR.get([128, TB], F32) for _ in range(2)]
            norm_T(nt, G_FFN1, hT)
            s = W.get([lambda e, t: e.dma_start(out=t[:, 0:128].rearrange("p (k n) -> p k n", n=8), in_=router_w.rearrange("(kc p) n -> p kc n", p=128))])
            wv = slots[s][:, 0:128].rearrange("p (k n) -> p k n", n=8)
            wk = f"ws{s}"
            for ti in range(nt):
                b = 4 + (ti % 2)
                r0 = ti * 16

                def mmr_(e, ti=ti, b=b, wv=wv):
                    for kc in range(16):
                        ins = e.matmul(psA[b][:, 0:8], lhsT=hT[:, kc, ti * 128:(ti + 1) * 128], rhs=wv[:, kc, :], start=(kc == 0), stop=(kc == 15))
                    return ins
                P.op("pe", mmr_, reads=[wk] + hT_keys(nt), writes=[f"psA{b}"])
                rk_ = f"rt{ti}"
                P.op("act", lambda e, b=b, r0=r0: e.activation(out=rt[:, r0:r0 + 8], in_=psA[b][:, 0:8], func=AF.Copy), reads=[f"psA{b}"], writes=[rk_])
                P.op("dve", lambda e, r0=r0: e.max(out=rt[:, r0 + 8:r0 + 16], in_=rt[:, r0:r0 + 8]), reads=[rk_], writes=[rk_])
                P.op("dve", lambda e, r0=r0, ti=ti: e.tensor_scalar(out=gates[:, ti, :], in0=rt[:, r0:r0 + 8], scalar1=rt[:, r0 + 9:r0 + 10], scalar2=0.0, op0=ALU.subtract, op1=ALU.is_ge),
                     reads=[rk_], writes=["gates"])
                P.op("dve", lambda e, r0=r0: e.tensor_scalar(out=rt[:, r0 + 10:r0 + 11], in0=rt[:, r0 + 8:r0 + 9], scalar1=-1.0, scalar2=None, op0=ALU.mult), reads=[rk_], writes=[rk_])
                P.op("act", lambda e, r0=r0: e.activation(out=rt[:, r0:r0 + 8], in_=rt[:, r0:r0 + 8], func=AF.Exp, bias=rt[:, r0 + 10:r0 + 11], scale=1.0), reads=[rk_], writes=[rk_])
                P.op("dve", lambda e, r0=r0, ti=ti: e.tensor_tensor(out=rt[:, r0:r0 + 8], in0=rt[:, r0:r0 + 8], in1=gates[:, ti, :], op=ALU.mult), reads=[rk_, "gates"], writes=[rk_])
                P.op("dve", lambda e, r0=r0: e.tensor_reduce(out=rt[:, r0 + 11:r0 + 12], in_=rt[:, r0:r0 + 8], axis=AX.X, op=ALU.add), reads=[rk_], writes=[rk_])
                P.op("dve", lambda e, r0=r0: e.reciprocal(out=rt[:, r0 + 12:r0 + 13], in_=rt[:, r0 + 11:r0 + 12]), reads=[rk_], writes=[rk_])
                P.op("dve", lambda e, r0=r0, ti=ti: e.tensor_scalar(out=gates[:, ti, :], in0=rt[:, r0:r0 + 8], scalar1=rt[:, r0 + 12:r0 + 13], scalar2=None, op0=ALU.mult), reads=[rk_], writes=["gates"])
            for ex in range(8):
                expert(hT, aT, sg, nt, moe_w_gate[ex], moe_w_up[ex], moe_w_down[ex], ex)
            P.barrier()
            if stop == "x4":
                dump(blk * TB, 4)
                return

            for ti in range(nt):
                rstd_of(ti)
                c = 8 * ti
                P.op("dve", lambda e, ti=ti, c=c: e.scalar_tensor_tensor(out=ot[:, :], in0=xacc[:, ti, :], scalar=st[:, c + 7:c + 8], in1=fgbc[:, :], op0=ALU.mult, op1=ALU.mult),
                     reads=[f"xacc{ti}", f"st{ti}", "fgbc"], writes=["ot"])
                kv = P.dma("sp", "ot", [lambda e, ti=ti, blk=blk: e.dma_start(out=out[blk * TB + ti * 128:blk * TB + (ti + 1) * 128, :], in_=ot[:, :])], reads=["ot"])
                final_kvs.append(kv)
            P.barrier()

        for blk in range(-1, NBLK):
            do_block(blk)
        P.wait_all("sp", [kv for kv in final_kvs if kv is not None])

    Pd = Prog(nc, dry=True)
    Wd = WStream(Pd, slots)
    program(Pd, Wd)
    P = Prog(nc)
    W = WStream(P, slots, sched=Wd.sched)
    program(P, W)
    P.emit()
    return nc


def _host_tables(half):
    inv_freq = (10000.0 ** (-np.arange(0, 64, 2, dtype=np.float32) / np.float32(64))).astype(np.float32)
    cf = np.zeros((64, 4), np.float32)
    cf[:, 0] = np.concatenate([inv_freq, inv_freq])
    cf[:, 1] = float(half * TOK)
    cf[:, 2] = math.pi
    cf[:, 3] = math.pi / 2
    pfix = np.ones((4, 16), np.float32)
    if half == 0:
        for g, w in enumerate((2, 4, 8, 16)):
            for t in range(16):
                pfix[g, t] = w / min(t + 1, w)
    pfix = np.ascontiguousarray(np.broadcast_to(pfix.reshape(1, 64), (128, 64))).astype(np.float32)
    pbias = np.full((128, 1), 0.0 if half == 1 else -30000.0, np.float32)
    return cf, pfix, pbias


_NC_CACHE = {}


def kernel(**inputs):
    stop = os.environ.get("KSTOP", "y")
    x = np.asarray(inputs["x"], dtype=np.float32)
    shared = {}
    for k, v in inputs.items():
        if k == "x":
            continue
        a = np.asarray(v, dtype=np.float32)
        if k != "final_norm":
            a = a[0]
        shared[k] = np.ascontiguousarray(a)
    shared["ident"] = np.eye(128, dtype=np.float32)
    shared["iota"] = np.ascontiguousarray(np.broadcast_to(np.arange(TB, dtype=np.float32), (64, TB)))
    shared["tri"] = np.triu(np.ones((128, 128), np.float32))
    in_maps = []
    for c in range(8):
        b, half = c // 2, c % 2
        cf, pfix, pbias = _host_tables(half)
        m = dict(shared)
        m["xo"] = np.ascontiguousarray(x[b, half * TOK:(half + 1) * TOK])
        m["xp"] = np.ascontiguousarray(x[b, 0:TOK]) if half == 1 else np.zeros((TOK, D), np.float32)
        m["cf"] = cf
        m["pfix"] = pfix
        m["pbias"] = pbias
        in_maps.append(m)
    if stop not in _NC_CACHE:
        _NC_CACHE[stop] = build(stop)
    nc = _NC_CACHE[stop]
    res = run_bass_kernel_spmd(nc, in_maps, core_ids=list(range(8)))
    y = np.empty((4, 4096, D), np.float32)
    for c in range(8):
        b, half = c // 2, c % 2
        y[b, half * TOK:(half + 1) * TOK] = res.results[c]["out"]
    return y
```

```python
import math
import os
import numpy as np
import concourse.bass as bass
import concourse.mybir as mybir
from concourse.bass_utils import run_bass_kernel_spmd

F32 = mybir.dt.float32
BF16 = mybir.dt.bfloat16
AF = mybir.ActivationFunctionType
ALU = mybir.AluOpType
AX = mybir.AxisListType

D = 2048
TOK = 2048
TB = 512
NBLK = int(os.environ.get("KNBLK", "4"))
EPS = 1e-6
SCALE = 192.0 ** -0.5
FE = 2816
NF = FE // 128
NS = 3
SLOT = 8192


class Prog:
    def __init__(self, nc, dry=False):
        self.nc = nc
        self.dry = dry
        self.ops = {e: [] for e in ("pe", "act", "dve", "pool", "sp")}
        self.sems = {}
        self.cnt = {}
        self.waited = {}
        self.lastw = {}
        self.readers = {}

    def sem(self, key):
        if key not in self.sems:
            self.sems[key] = self.nc.alloc_semaphore("s_" + key.replace(":", "_"))
            self.cnt[key] = 0
        return self.sems[key]

    def _deps(self, eng, reads, writes, extra=()):
        deps = {}

        def add(kv):
            if kv[1] > deps.get(kv[0], 0):
                deps[kv[0]] = kv[1]

        for r in reads:
            if r in self.lastw:
                add(self.lastw[r])
        for w in writes:
            if w in self.lastw:
                add(self.lastw[w])
            for kv in self.readers.get(w, ()):
                add(kv)
        for kv in extra:
            add(kv)
        waits = []
        for k, v in deps.items():
            if k == eng and eng == "pe":
                continue
            if self.waited.get((eng, k), 0) >= v:
                continue
            self.waited[(eng, k)] = v
            waits.append((self.sems[k], v))
        return waits

    def _commit(self, kv, reads, writes):
        for r in reads:
            self.readers.setdefault(r, []).append(kv)
        for w in writes:
            self.lastw[w] = kv
            self.readers[w] = []

    def op(self, eng, fn, reads=(), writes=()):
        if self.dry:
            return None
        s = self.sem(eng)
        waits = self._deps(eng, reads, writes)
        self.cnt[eng] += 1
        kv = (eng, self.cnt[eng])
        self._commit(kv, reads, writes)

        def f(e, waits=waits, fn=fn, s=s):
            for (ws, wv) in waits:
                e.wait_ge(ws, wv)
            fn(e).then_inc(s, 1)

        self.ops[eng].append(f)
        return kv

    def dma(self, eng, semkey, fns, reads=(), writes=()):
        if self.dry:
            return None
        key = "d:" + semkey
        s = self.sem(key)
        extra = [(key, self.cnt[key])] if self.cnt[key] > 0 else []
        waits = self._deps(eng, reads, writes, extra)
        self.cnt[key] += 16 * len(fns)
        kv = (key, self.cnt[key])
        self._commit(kv, reads, writes)

        def f(e, waits=waits, fns=fns, s=s):
            for (ws, wv) in waits:
                e.wait_ge(ws, wv)
            for fn in fns:
                fn(e).then_inc(s, 16)

        self.ops[eng].append(f)
        return kv

    def barrier(self, engs=("pe", "act", "dve")):
        if self.dry:
            return
        for e in engs:
            waits = []
            for k in engs:
                if k == e or k not in self.cnt:
                    continue
                v = self.cnt[k]
                if self.waited.get((e, k), 0) >= v:
                    continue
                self.waited[(e, k)] = v
                waits.append((self.sems[k], v))
            if waits:
                self.ops[e].append(lambda en, waits=waits: [en.wait_ge(ws, wv) for (ws, wv) in waits])

    def wait_all(self, eng, kvs):
        if self.dry:
            return
        waits = []
        for k, v in kvs:
            if self.waited.get((eng, k), 0) >= v:
                continue
            self.waited[(eng, k)] = v
            waits.append((self.sems[k], v))
        self.ops[eng].append(lambda en, waits=waits: [en.wait_ge(ws, wv) for (ws, wv) in waits])

    def emit(self):
        with self.nc.Block() as block:
            @block.tensor
            def _(e):
                for f in self.ops["pe"]:
                    f(e)

            @block.scalar
            def _(e):
                for f in self.ops["act"]:
                    f(e)

            @block.vector
            def _(e):
                for f in self.ops["dve"]:
                    f(e)

            @block.gpsimd
            def _(e):
                for f in self.ops["pool"]:
                    f(e)

            @block.sync
            def _(e):
                for f in self.ops["sp"]:
                    f(e)


class WStream:
    def __init__(self, P, slots, sched=None):
        self.P = P
        self.slots = slots
        self.dry = sched is None
        self.sched = [] if sched is None else sched
        self.i = 0
        self.issued = 0

    def get(self, fns):
        if self.dry:
            self.sched.append(fns)
            return (len(self.sched) - 1) % NS
        i = self.i
        self.i += 1
        while self.issued < min(len(self.sched), i + NS):
            j = self.issued
            s = j % NS
            t = self.slots[s]
            self.P.dma("pool", f"ws{s}", [(lambda e, f=f, t=t: f(e, t)) for f in self.sched[j]], writes=[f"ws{s}"])
            self.issued += 1
        return i % NS


class Arena:
    def __init__(self, t, nbytes):
        self.t = t
        self.n = nbytes
        self.off = 0

    def reset(self):
        self.off = 0

    def get(self, shape, dtype):
        esz = 4 if dtype == F32 else 2
        nb = int(np.prod(shape[1:])) * esz
        nb_al = (nb + 63) // 64 * 64
        assert self.off + nb_al <= self.n, (self.off, nb_al, self.n)
        ap = self.t[0:shape[0], self.off // 4:(self.off + nb) // 4]
        if dtype != F32:
            ap = ap.bitcast(dtype)
        if len(shape) == 3:
            ap = ap.rearrange("p (a b) -> p a b", b=shape[2])
        self.off += nb_al
        return ap


ARENA_BYTES = 47 * 1024


def build(stop="y"):
    nc = bass.Bass("TRN2", target_bir_lowering=False)

    def din(name, shape):
        return nc.dram_tensor(name, list(shape), F32, kind="ExternalInput").ap()

    xo = din("xo", [TOK, D])
    xp = din("xp", [TOK, D])
    pbias_d = din("pbias", [128, 1])
    iota_d = din("iota", [64, TB])
    cf_d = din("cf", [64, 4])
    pfix_d = din("pfix", [128, 64])
    ident_d = din("ident", [128, 128])
    tri_d = din("tri", [128, 128])
    norm_mix0 = din("norm_mix0", [D])
    w_in = din("w_in", [D, 3904])
    conv_w = din("conv_w", [3, 1024])
    q_norm = din("q_norm", [512])
    w_uq = din("w_uq", [512, 1536])
    kv_norm = din("kv_norm", [256])
    w_ukv = din("w_ukv", [256, 2048])
    w_out = din("w_out", [D, D])
    norm_ffn0 = din("norm_ffn0", [D])
    ffn_w_gate = din("ffn_w_gate", [D, 5632])
    ffn_w_up = din("ffn_w_up", [D, 5632])
    ffn_w_down = din("ffn_w_down", [5632, D])
    norm_mix1 = din("norm_mix1", [D])
    pool_w = din("pool_w", [4, 512, 512])
    pool_scale = din("pool_scale", [D])
    norm_ffn1 = din("norm_ffn1", [D])
    router_w = din("router_w", [D, 8])
    moe_w_gate = din("moe_w_gate", [8, D, FE])
    moe_w_up = din("moe_w_up", [8, D, FE])
    moe_w_down = din("moe_w_down", [8, FE, D])
    final_norm = din("final_norm", [D])
    out = nc.dram_tensor("out", [TOK, D], F32, kind="ExternalOutput").ap()

    sb = nc.alloc_sbuf_tensor
    xacc = sb("xacc", [128, 4, D], F32)
    YT = sb("YT", [128, 16, TB], BF16)
    ckvnT = sb("ckvnT", [128, 2, 4096], BF16)
    krT = sb("krT", [64, 4096], BF16)
    cqnT = sb("cqnT", [128, 4, TB], BF16)
    slots = [sb(f"wslot{i}", [128, SLOT], BF16) for i in range(NS)]
    hb = sb("hb", [128, D], BF16)
    cs_sb = sb("cs_sb", [64, 2, TB], F32)
    psc = sb("psc", [128, 512], F32)
    fgbc = sb("fgbc", [128, D], F32)
    ot = sb("ot", [128, D], F32)
    iot = sb("iot", [64, TB], F32)
    cfs = sb("cfs", [64, 4], F32)
    gT = sb("gT", [128, 128], F32)
    identb = sb("identb", [128, 128], BF16)
    onesb = sb("onesb", [128, 128], BF16)
    trib = sb("trib", [128, 128], BF16)
    wkrrot = sb("wkrrot", [128, 16, 64], BF16)
    wqrot = sb("wqrot", [128, 4, 64], BF16)
    uh = sb("uh", [128, 8, 2], F32)
    h1halo = sb("h1halo", [128, 16, 16], BF16)
    pbias = sb("pbias_sb", [128, 1], F32)
    zbias = sb("zbias", [128, 1], F32)
    pfix = sb("pfix_sb", [128, 64], F32)
    st = sb("st", [128, 32], F32)
    junk = sb("junk", [128, 512], BF16)
    rt = sb("rt", [128, 64], F32)
    gates = sb("gates", [128, 4, 8], F32)
    arena_t = sb("arena", [128, ARENA_BYTES // 4], F32)
    AR = Arena(arena_t, ARENA_BYTES)

    psT = [nc.alloc_psum_tensor(f"psT{i}", [128, 8, 128], BF16) for i in range(2)]
    psA = [nc.alloc_psum_tensor(f"psA{i}", [128, 512], F32) for i in range(6)]

    def kcv(ap2d):
        return ap2d.rearrange("(kc p) n -> p kc n", p=128)

    def program(P, W):
        final_kvs = []
        AR.reset()
        gst = AR.get([128, 128], F32)
        identf = AR.get([128, 128], F32)
        trif = AR.get([128, 128], F32)
        P.dma("sp", "c6", [lambda e: e.dma_start(out=iot[:, :], in_=iota_d[:, :])], writes=["iot"])
        P.dma("sp", "c7", [lambda e: e.dma_start(out=cfs[:, :], in_=cf_d[:, :])], writes=["cfs"])

        P.dma("sp", "c0", [lambda e: e.dma_start(out=identf[:, :], in_=ident_d[:, :])], writes=["identf"])
        P.dma("sp", "c1", [lambda e: e.dma_start(out=trif[:, :], in_=tri_d[:, :])], writes=["trif"])
        P.dma("sp", "c2", [lambda e: e.dma_start(out=pbias[:, :], in_=pbias_d[:, :])], writes=["pbias"])
        P.dma("sp", "c3", [lambda e: e.dma_start(out=pfix[:, :], in_=pfix_d[:, :])], writes=["pfix"])
        P.dma("sp", "c4", [lambda e: e.dma_start(out=fgbc[:, :], in_=final_norm.rearrange("(o n) -> o n", o=1).to_broadcast([128, D]))], writes=["fgbc"])
        P.op("dve", lambda e: e.memset(gst[:, :], 0.0), writes=["gst"])
        P.op("dve", lambda e: e.memset(zbias[:, :], 0.0), writes=["zbias"])
        P.op("dve", lambda e: e.memset(uh[:, :, :], 0.0), writes=["uh"])
        P.op("dve", lambda e: e.memset(h1halo[:, :, :], 0.0), writes=["h1halo"])
        P.op("dve", lambda e: e.memset(onesb[:, :], 1.0), writes=["onesb"])
        vecs = [(norm_mix0, 0, 16), (norm_ffn0, 16, 16), (norm_mix1, 32, 16), (norm_ffn1, 48, 16), (q_norm, 64, 4), (kv_norm, 68, 2)]
        fns = [(lambda e, v=v, r=r, n=n: e.dma_start(out=gst[r:r + n, :], in_=v.rearrange("(c p) -> c p", p=128))) for (v, r, n) in vecs]
        fns.append(lambda e: e.dma_start(out=gst[70:94, :], in_=conv_w.rearrange("j (c p) -> (j c) p", p=128)))
        P.dma("sp", "c5", fns, writes=["gst"])
        P.op("dve", lambda e: e.tensor_copy(out=identb[:, :], in_=identf[:, :]), reads=["identf"], writes=["identb"])
        P.op("dve", lambda e: e.tensor_copy(out=trib[:, :], in_=trif[:, :]), reads=["trif"], writes=["trib"])
        P.op("pe", lambda e: e.transpose(out=psA[0][:, 0:128], in_=gst[:, :], identity=identf[:, :]), reads=["gst", "identf"], writes=["psA0"])
        P.op("dve", lambda e: e.tensor_copy(out=gT[:, :], in_=psA[0][:, 0:128]), reads=["psA0"], writes=["gT"])
        G_MIX0, G_FFN0, G_MIX1, G_FFN1, G_Q, G_KV, G_CONV = 0, 16, 32, 48, 64, 68, 70

        def load_x(src, row0, nt):
            for ti in range(nt):
                P.dma("sp", f"x{ti}", [lambda e, ti=ti: e.dma_start(out=xacc[:, ti, :], in_=src[row0 + ti * 128:row0 + (ti + 1) * 128, :])],
                      writes=[f"xacc{ti}"])

        def rstd_of(ti):
            c = 8 * ti
            for q in range(4):
                P.op("act", lambda e, q=q: e.activation(out=junk[:, :], in_=xacc[:, ti, q * 512:(q + 1) * 512], func=AF.Square, accum_out=st[:, c + q:c + q + 1]),
                     reads=[f"xacc{ti}"], writes=[f"st{ti}q{q}", "junk"])
            P.op("dve", lambda e: e.tensor_reduce(out=st[:, c + 4:c + 5], in_=st[:, c:c + 4], axis=AX.X, op=ALU.add),
                 reads=[f"st{ti}q{q}" for q in range(4)], writes=[f"st{ti}"])
            P.op("dve", lambda e: e.tensor_scalar(out=st[:, c + 5:c + 6], in0=st[:, c + 4:c + 5], scalar1=1.0 / D, scalar2=EPS, op0=ALU.mult, op1=ALU.add),
                 reads=[f"st{ti}"], writes=[f"st{ti}"])
            P.op("act", lambda e: e.activation(out=st[:, c + 6:c + 7], in_=st[:, c + 5:c + 6], func=AF.Sqrt), reads=[f"st{ti}"], writes=[f"st{ti}"])
            P.op("dve", lambda e: e.reciprocal(out=st[:, c + 7:c + 8], in_=st[:, c + 6:c + 7]), reads=[f"st{ti}"], writes=[f"st{ti}"])

        def norm_T(nt, grow, hT, col0=0):
            for ti in range(nt):
                rstd_of(ti)
                c = 8 * ti
                for half in range(2):
                    P.op("dve", lambda e, ti=ti, c=c, half=half: e.tensor_scalar(out=hb[:, half * 1024:(half + 1) * 1024], in0=xacc[:, ti, half * 1024:(half + 1) * 1024],
                                                                                 scalar1=st[:, c + 7:c + 8], scalar2=None, op0=ALU.mult),
                         reads=[f"xacc{ti}", f"st{ti}"], writes=[f"hb{half}"])
                    for g4 in (2 * half, 2 * half + 1):
                        pt = psT[g4 % 2]

                        def tr(e, g4=g4, pt=pt):
                            for j in range(4):
                                ins = e.transpose(out=pt[:, j, :], in_=hb[:, (g4 * 4 + j) * 128:(g4 * 4 + j + 1) * 128], identity=identb[:, :])
                            return ins
                        P.op("pe", tr, reads=[f"hb{half}", "identb"], writes=[f"psT{g4 % 2}"])
                        c0 = col0 + ti * 128
                        if g4 % 2 == 0:
                            P.op("dve", lambda e, g4=g4, pt=pt, c0=c0: e.tensor_tensor(
                                out=hT[:, g4 * 4:(g4 + 1) * 4, c0:c0 + 128], in0=pt[:, 0:4, :],
                                in1=gT[:, grow + g4 * 4:grow + g4 * 4 + 4].unsqueeze(2).to_broadcast([128, 4, 128]), op=ALU.mult),
                                reads=[f"psT{g4 % 2}", "gT"], writes=[f"hT{ti}"])
                        else:
                            def ev(e, g4=g4, pt=pt, c0=c0):
                                for j in range(4):
                                    kc = g4 * 4 + j
                                    ins = e.mul(out=hT[:, kc, c0:c0 + 128], in_=pt[:, j, :], mul=gT[:, grow + kc:grow + kc + 1])
                                return ins
                            P.op("act", ev, reads=[f"psT{g4 % 2}", "gT"], writes=[f"hT{ti}"])

        def hT_keys(nt):
            return [f"hT{ti}" for ti in range(nt)]

        def lat_norm(ps_list, nchunk, n, raw, sq, rk, gcol, dst_fn, dst_key, inv_dim):
            for j in range(nchunk):
                P.op("act", lambda e, j=j: e.activation(out=raw[:, j, 0:n], in_=ps_list[j][1][:, 0:n], func=AF.Copy), reads=[ps_list[j][0]], writes=[f"raw{j}"])
                P.op("act", lambda e, j=j: e.activation(out=sq[:, j, 0:n], in_=ps_list[j][1][:, 0:n], func=AF.Square), reads=[ps_list[j][0]], writes=[f"sq{j}"])

            def mm(e):
                for j in range(nchunk):
                    ins = e.matmul(psA[5][:, 0:n], lhsT=onesb[:, :], rhs=sq[:, j, 0:n], start=(j == 0), stop=(j == nchunk - 1))
                return ins
            P.op("pe", mm, reads=[f"sq{j}" for j in range(nchunk)] + ["onesb"], writes=["psA5"])
            P.op("dve", lambda e: e.tensor_scalar(out=rk[:, 0:n], in0=psA[5][:, 0:n], scalar1=inv_dim, scalar2=EPS, op0=ALU.mult, op1=ALU.add), reads=["psA5"], writes=["rk"])
            P.op("act", lambda e: e.activation(out=rk[:, 0:n], in_=rk[:, 0:n], func=AF.Sqrt), reads=["rk"], writes=["rk"])
            P.op("dve", lambda e: e.reciprocal(out=rk[:, 0:n], in_=rk[:, 0:n]), reads=["rk"], writes=["rk"])
            for j in range(nchunk):
                P.op("dve", lambda e, j=j: e.scalar_tensor_tensor(out=dst_fn(j), in0=raw[:, j, 0:n], scalar=gT[:, gcol + j:gcol + j + 1], in1=rk[:, 0:n], op0=ALU.mult, op1=ALU.mult),
                     reads=[f"raw{j}", "rk", "gT"], writes=[dst_key])

        MAGIC = 12582912.0
        C1 = 6.28125
        C2 = 2.0 * math.pi - 6.28125

        def rope_tables(n, p0, own, tmp1, tmp2, tmp3):
            ang, kk, rr = tmp1[0:64, 0:n], tmp2[0:64, 0:n], tmp3[0:64, 0:n]
            if own:
                P.op("dve", lambda e: e.tensor_scalar(out=ang, in0=iot[:, 0:n], scalar1=cfs[:, 1:2], scalar2=float(p0), op0=ALU.add, op1=ALU.add), reads=["iot", "cfs"], writes=["tmp1"])
            else:
                P.op("dve", lambda e: e.tensor_scalar(out=ang, in0=iot[:, 0:n], scalar1=float(p0), scalar2=None, op0=ALU.add), reads=["iot"], writes=["tmp1"])
            P.op("dve", lambda e: e.tensor_scalar(out=ang, in0=ang, scalar1=cfs[:, 0:1], scalar2=None, op0=ALU.mult), reads=["tmp1", "cfs"], writes=["tmp1"])
            P.op("dve", lambda e: e.tensor_scalar(out=kk, in0=ang, scalar1=1.0 / (2.0 * math.pi), scalar2=MAGIC, op0=ALU.mult, op1=ALU.add), reads=["tmp1"], writes=["tmp2"])
            P.op("dve", lambda e: e.tensor_scalar(out=kk, in0=kk, scalar1=MAGIC, scalar2=None, op0=ALU.subtract), reads=["tmp2"], writes=["tmp2"])
            P.op("dve", lambda e: e.scalar_tensor_tensor(out=rr, in0=kk, scalar=-C1, in1=ang, op0=ALU.mult, op1=ALU.add), reads=["tmp2", "tmp1"], writes=["tmp3"])
            P.op("dve", lambda e: e.scalar_tensor_tensor(out=rr, in0=kk, scalar=-C2, in1=rr, op0=ALU.mult, op1=ALU.add), reads=["tmp2", "tmp3"], writes=["tmp3"])
            P.op("dve", lambda e: e.tensor_scalar(out=rr, in0=rr, scalar1=-math.pi, scalar2=math.pi, op0=ALU.max, op1=ALU.min), reads=["tmp3"], writes=["tmp3"])
            P.op("act", lambda e: e.activation(out=cs_sb[:, 1, 0:n], in_=rr, func=AF.Sin), reads=["tmp3"], writes=["cs_sb"])
            P.op("dve", lambda e: e.scalar_tensor_tensor(out=kk, in0=rr, scalar=-1.0, in1=rr, op0=ALU.mult, op1=ALU.max), reads=["tmp3"], writes=["tmp2"])
            P.op("act", lambda e: e.activation(out=cs_sb[:, 0, 0:n], in_=kk, func=AF.Sin, bias=cfs[:, 3:4], scale=-1.0), reads=["tmp2", "cfs"], writes=["cs_sb"])

        def kv_latents(hT, nt, col0, own, p0, raw, sq, rk, tmp1, tmp2, tmp3):
            n = nt * 128
            s = W.get([lambda e, t: e.dma_start(out=t[:, 0:16 * 320].rearrange("p (k n) -> p k n", n=320), in_=kcv(w_in)[:, :, 3584:3904])])
            wv = slots[s][:, 0:16 * 320].rearrange("p (k n) -> p k n", n=320)
            wk = f"ws{s}"
            P.op("dve", lambda e: e.tensor_scalar(out=wkrrot[:, :, 0:32], in0=wv[:, :, 288:320], scalar1=-1.0, scalar2=None, op0=ALU.mult), reads=[wk], writes=["wkrrot"])
            P.op("dve", lambda e: e.tensor_copy(out=wkrrot[:, :, 32:64], in_=wv[:, :, 256:288]), reads=[wk], writes=["wkrrot"])
            rope_tables(n, p0, own, tmp1, tmp2, tmp3)
            for j in range(2):
                def mm(e, j=j):
                    for kc in range(16):
                        ins = e.matmul(psA[j][:, 0:n], lhsT=wv[:, kc, j * 128:(j + 1) * 128], rhs=hT[:, kc, 0:n], start=(kc == 0), stop=(kc == 15))
                    return ins
                P.op("pe", mm, reads=[wk] + hT_keys(nt), writes=[f"psA{j}"])
            lat_norm([("psA0", psA[0]), ("psA1", psA[1])], 2, n, raw, sq, rk, G_KV, lambda j: ckvnT[:, j, col0:col0 + n], "ckvnT", 1.0 / 256)

            def mmr(e):
                for kc in range(16):
                    ins = e.matmul(psA[2][0:64, 0:n], lhsT=wv[:, kc, 256:320], rhs=hT[:, kc, 0:n], start=(kc == 0), stop=(kc == 15))
                return ins
            P.op("pe", mmr, reads=[wk] + hT_keys(nt), writes=["psA2"])

            def mmr2(e):
                for kc in range(16):
                    ins = e.matmul(psA[3][0:64, 0:n], lhsT=wkrrot[:, kc, :], rhs=hT[:, kc, 0:n], start=(kc == 0), stop=(kc == 15))
                return ins
            P.op("pe", mmr2, reads=["wkrrot"] + hT_keys(nt), writes=["psA3"])
            P.op("dve", lambda e: e.tensor_tensor(out=tmp1[0:64, 0:n], in0=psA[2][0:64, 0:n], in1=cs_sb[:, 0, 0:n], op=ALU.mult), reads=["psA2", "cs_sb"], writes=["tmp1"])
            P.op("dve", lambda e: e.tensor_tensor(out=tmp2[0:64, 0:n], in0=psA[3][0:64, 0:n], in1=cs_sb[:, 1, 0:n], op=ALU.mult), reads=["psA3", "cs_sb"], writes=["tmp2"])
            P.op("dve", lambda e: e.tensor_tensor(out=krT[:, col0:col0 + n], in0=tmp1[0:64, 0:n], in1=tmp2[0:64, 0:n], op=ALU.add), reads=["tmp1", "tmp2"], writes=["krT"])

        def m1_alloc():
            AR.reset()
            hT = AR.get([128, 16, TB], BF16)
            raw = AR.get([128, 4, TB], F32)
            sq = AR.get([128, 4, TB], BF16)
            rk = AR.get([128, TB], F32)
            tmp1 = AR.get([128, TB], F32)
            tmp2 = AR.get([128, TB], F32)
            tmp3 = AR.get([128, TB], F32)
            return hT, raw, sq, rk, tmp1, tmp2, tmp3

        P.barrier()
        hT, raw, sq, rk, tmp1, tmp2, tmp3 = m1_alloc()
        for g in range(4):
            load_x(xp, g * 512, 4)
            norm_T(4, G_MIX0, hT)
            kv_latents(hT, 4, g * 512, False, g * 512, raw, sq, rk, tmp1, tmp2, tmp3)
        P.barrier()

        def expert(hT, aT, sg, nt, wg, wu, wd, gate_e):
            n = nt * 128
            for fp in range(NF // 2):
                s = W.get([lambda e, t, fp=fp: e.dma_start(out=t[:, 0:4096].rearrange("p (k n) -> p k n", n=256), in_=kcv(wg)[:, :, fp * 256:(fp + 1) * 256]),
                           lambda e, t, fp=fp: e.dma_start(out=t[:, 4096:8192].rearrange("p (k n) -> p k n", n=256), in_=kcv(wu)[:, :, fp * 256:(fp + 1) * 256])])
                wgv = slots[s][:, 0:4096].rearrange("p (k n) -> p k n", n=256)
                wuv = slots[s][:, 4096:8192].rearrange("p (k n) -> p k n", n=256)
                wk = f"ws{s}"
                for f2 in range(2):
                    f = fp * 2 + f2
                    bg = (f % 2) * 2
                    pg, pu = psA[bg], psA[bg + 1]

                    def mmg(e, f2=f2, pg=pg, wgv=wgv):
                        for kc in range(16):
                            ins = e.matmul(pg[:, 0:n], lhsT=wgv[:, kc, f2 * 128:(f2 + 1) * 128], rhs=hT[:, kc, 0:n], start=(kc == 0), stop=(kc == 15))
                        return ins

                    def mmu(e, f2=f2, pu=pu, wuv=wuv):
                        for kc in range(16):
                            ins = e.matmul(pu[:, 0:n], lhsT=wuv[:, kc, f2 * 128:(f2 + 1) * 128], rhs=hT[:, kc, 0:n], start=(kc == 0), stop=(kc == 15))
                        return ins
                    P.op("pe", mmg, reads=[wk] + hT_keys(nt), writes=[f"psA{bg}"])
                    P.op("pe", mmu, reads=[wk] + hT_keys(nt), writes=[f"psA{bg + 1}"])
                    sgb = sg[f % 2]
                    P.op("act", lambda e, pg=pg, sgb=sgb: e.activation(out=sgb[:, 0:n], in_=pg[:, 0:n], func=AF.Silu), reads=[f"psA{bg}"], writes=[f"sg{f % 2}"])
                    P.op("dve", lambda e, pu=pu, sgb=sgb, f=f: e.tensor_tensor(out=aT[:, f, 0:n], in0=pu[:, 0:n], in1=sgb[:, 0:n], op=ALU.mult),
                         reads=[f"psA{bg + 1}", f"sg{f % 2}"], writes=[f"aT{f}"])
            cnt = 0
            for c8 in range(8):
                s = W.get([lambda e, t, c8=c8: e.dma_start(out=t[:, 0:NF * 256].rearrange("p (k n) -> p k n", n=256), in_=wd.rearrange("(f p) n -> p f n", p=128)[:, :, c8 * 256:(c8 + 1) * 256])])
                wdv = slots[s][:, 0:NF * 256].rearrange("p (k n) -> p k n", n=256)
                wk = f"ws{s}"
                for ti in range(nt):
                    b = 4 + (cnt % 2)
                    cnt += 1
                    pd = psA[b]

                    def mmd(e, ti=ti, pd=pd, wdv=wdv):
                        for f in range(NF):
                            ins = e.matmul(pd[:, 0:256], lhsT=aT[:, f, ti * 128:(ti + 1) * 128], rhs=wdv[:, f, :], start=(f == 0), stop=(f == NF - 1))
                        return ins
                    P.op("pe", mmd, reads=[wk] + [f"aT{f}" for f in range(NF)], writes=[f"psA{b}"])
                    xs_ = xacc[:, ti, c8 * 256:(c8 + 1) * 256]
                    if gate_e is None:
                        P.op("dve", lambda e, pd=pd, xs_=xs_: e.tensor_tensor(out=xs_, in0=pd[:, 0:256], in1=xs_, op=ALU.add),
                             reads=[f"psA{b}", f"xacc{ti}"], writes=[f"xacc{ti}"])
                    else:
                        P.op("dve", lambda e, pd=pd, xs_=xs_, ti=ti: e.scalar_tensor_tensor(out=xs_, in0=pd[:, 0:256], scalar=gates[:, ti, gate_e:gate_e + 1], in1=xs_, op0=ALU.mult, op1=ALU.add),
                             reads=[f"psA{b}", f"xacc{ti}", "gates"], writes=[f"xacc{ti}"])

        def dump(row0, nt):
            for ti in range(nt):
                kv = P.dma("sp", f"o{ti}", [lambda e, ti=ti: e.dma_start(out=out[row0 + ti * 128:row0 + (ti + 1) * 128, :], in_=xacc[:, ti, :])], reads=[f"xacc{ti}"])
                final_kvs.append(kv)

        def do_block(blk):
            halo = blk < 0
            nt = 1 if halo else 4
            n = nt * 128
            q0 = 15 if halo else 16 + 4 * blk
            nk = q0 + nt
            hT, raw, sq, rk, tmp1, tmp2, tmp3 = m1_alloc()
            u = AR.get([128, TB + 2], F32)
            tb = AR.get([128, TB], F32)
            xs = AR.get([128, TB], F32)
            gbs = AR.get([128, TB], F32)
            if halo:
                load_x(xp, 1920, 1)
            else:
                load_x(xo, blk * TB, 4)
            norm_T(nt, G_MIX0, hT)
            for c in range(8):
                s = W.get([(lambda e, t, c=c, j=j: e.dma_start(out=t[:, 0:6144].rearrange("p (k j n) -> p k j n", j=3, n=128)[:, :, j, :],
                                                               in_=kcv(w_in)[:, :, j * 1024 + c * 128:j * 1024 + (c + 1) * 128])) for j in range(3)])
                wv = slots[s][:, 0:6144].rearrange("p (k j n) -> p k j n", j=3, n=128)
                wk = f"ws{s}"
                for j in range(3):
                    def mm(e, j=j, wv=wv):
                        for kc in range(16):
                            ins = e.matmul(psA[j][:, 0:n], lhsT=wv[:, kc, j, :], rhs=hT[:, kc, 0:n], start=(kc == 0), stop=(kc == 15))
                        return ins
                    P.op("pe", mm, reads=[wk] + hT_keys(nt), writes=[f"psA{j}"])
                P.op("act", lambda e: e.activation(out=xs[:, 0:n], in_=psA[0][:, 0:n], func=AF.Copy), reads=["psA0"], writes=["xs"])
                P.op("act", lambda e: e.activation(out=gbs[:, 0:n], in_=psA[1][:, 0:n], func=AF.Copy), reads=["psA1"], writes=["gbs"])
                P.op("dve", lambda e, c=c: e.tensor_copy(out=u[:, 0:2], in_=uh[:, c, :]), reads=["uh"], writes=["u"])
                P.op("dve", lambda e: e.tensor_tensor(out=u[:, 2:2 + n], in0=psA[2][:, 0:n], in1=xs[:, 0:n], op=ALU.mult), reads=["psA2", "xs"], writes=["u"])
                P.op("dve", lambda e, c=c: e.tensor_scalar(out=tb[:, 0:n], in0=u[:, 2:2 + n], scalar1=gT[:, G_CONV + 16 + c:G_CONV + 17 + c], scalar2=None, op0=ALU.mult),
                     reads=["u", "gT"], writes=["tb"])
                P.op("dve", lambda e, c=c: e.scalar_tensor_tensor(out=tb[:, 0:n], in0=u[:, 1:1 + n], scalar=gT[:, G_CONV + 8 + c:G_CONV + 9 + c], in1=tb[:, 0:n], op0=ALU.mult, op1=ALU.add),
                     reads=["u", "gT", "tb"], writes=["tb"])
                P.op("dve", lambda e, c=c: e.scalar_tensor_tensor(out=tb[:, 0:n], in0=u[:, 0:n], scalar=gT[:, G_CONV + c:G_CONV + 1 + c], in1=tb[:, 0:n], op0=ALU.mult, op1=ALU.add),
                     reads=["u", "gT", "tb"], writes=["tb"])
                P.op("dve", lambda e, c=c: e.tensor_copy(out=uh[:, c, :], in_=u[:, n:n + 2]), reads=["u"], writes=["uh"])
                P.op("dve", lambda e, c=c: e.tensor_tensor(out=YT[:, c, 0:n], in0=tb[:, 0:n], in1=gbs[:, 0:n], op=ALU.mult), reads=["tb", "gbs"], writes=[f"YT{c}"])
            s = W.get([lambda e, t: e.dma_start(out=t[:, :].rearrange("p (k n) -> p k n", n=512), in_=kcv(w_in)[:, :, 3072:3584])])
            wv = slots[s][:, :].rearrange("p (k n) -> p k n", n=512)
            wk = f"ws{s}"
            for j in range(4):
                def mm(e, j=j, wv=wv):
                    for kc in range(16):
                        ins = e.matmul(psA[j][:, 0:n], lhsT=wv[:, kc, j * 128:(j + 1) * 128], rhs=hT[:, kc, 0:n], start=(kc == 0), stop=(kc == 15))
                    return ins
                P.op("pe", mm, reads=[wk] + hT_keys(nt), writes=[f"psA{j}"])
            lat_norm([(f"psA{j}", psA[j]) for j in range(4)], 4, n, raw, sq, rk, G_Q, lambda j: cqnT[:, j, 0:n], "cqnT", 1.0 / 512)
            if not halo:
                kv_latents(hT, 4, 2048 + blk * TB, True, blk * TB, raw, sq, rk, tmp1, tmp2, tmp3)
            else:
                rope_tables(128, 1920, False, tmp1, tmp2, tmp3)
            P.barrier()

            AR.reset()
            HB = []
            for i in range(2):
                HB.append(dict(KnT=AR.get([128, 4096], BF16), Vh=AR.get([128, 32, 128], BF16), QnT=AR.get([128, TB], BF16), qrT=AR.get([64, TB], BF16)))
            PT = [AR.get([128, TB], BF16) for _ in range(3)]
            rs = AR.get([128, TB], F32)
            t1 = AR.get([128, TB], F32)
            t2 = AR.get([128, TB], F32)

            def gen(h):
                hs = h % 2
                KnT, Vh, QnT, qrT = HB[hs]["KnT"], HB[hs]["Vh"], HB[hs]["QnT"], HB[hs]["qrT"]
                s = W.get([lambda e, t, h=h: e.dma_start(out=t[:, 0:768].rearrange("p (k n) -> p k n", n=192), in_=kcv(w_uq)[:, :, h * 192:(h + 1) * 192]),
                           lambda e, t, h=h: e.dma_start(out=t[:, 1024:1536].rearrange("p (k n) -> p k n", n=256), in_=kcv(w_ukv)[:, :, h * 256:(h + 1) * 256])])
                wq = slots[s][:, 0:768].rearrange("p (k n) -> p k n", n=192)
                wkv = slots[s][:, 1024:1536].rearrange("p (k n) -> p k n", n=256)
                wk = f"ws{s}"
                P.op("dve", lambda e: e.tensor_scalar(out=wqrot[:, :, 0:32], in0=wq[:, :, 160:192], scalar1=-1.0, scalar2=None, op0=ALU.mult), reads=[wk], writes=["wqrot"])
                P.op("dve", lambda e: e.tensor_copy(out=wqrot[:, :, 32:64], in_=wq[:, :, 128:160]), reads=[wk], writes=["wqrot"])
                ngk = (nk * 128 + 511) // 512
                for kg in range(ngk):
                    c0 = kg * 512
                    cn = min(512, nk * 128 - c0)
                    b = 4 + (kg % 2)

                    def mmk(e, c0=c0, cn=cn, b=b):
                        for kc in range(2):
                            ins = e.matmul(psA[b][:, 0:cn], lhsT=wkv[:, kc, 0:128], rhs=ckvnT[:, kc, c0:c0 + cn], start=(kc == 0), stop=(kc == 1))
                        return ins
                    P.op("pe", mmk, reads=[wk, "ckvnT"], writes=[f"psA{b}"])
                    P.op("act", lambda e, c0=c0, cn=cn, b=b: e.activation(out=KnT[:, c0:c0 + cn], in_=psA[b][:, 0:cn], func=AF.Copy), reads=[f"psA{b}"], writes=[f"KnT{hs}"])
                for vg in range((nk + 3) // 4):
                    k0 = vg * 4
                    kn = min(4, nk - k0)
                    b = 4 + (vg % 2)

                    def mmv(e, k0=k0, kn=kn, b=b):
                        for j in range(kn):
                            for kc in range(2):
                                ins = e.matmul(psA[b][:, j * 128:(j + 1) * 128], lhsT=ckvnT[:, kc, (k0 + j) * 128:(k0 + j + 1) * 128], rhs=wkv[:, kc, 128:256], start=(kc == 0), stop=(kc == 1))
                        return ins
                    P.op("pe", mmv, reads=[wk, "ckvnT"], writes=[f"psA{b}"])
                    P.op("dve", lambda e, k0=k0, kn=kn, b=b: e.tensor_copy(out=Vh[:, k0:k0 + kn, :], in_=psA[b][:, 0:kn * 128].rearrange("p (a b) -> p a b", b=128)),
                         reads=[f"psA{b}"], writes=[f"Vh{hs}"])

                def mmq(e):
                    for kc in range(4):
                        ins = e.matmul(psA[4][:, 0:n], lhsT=wq[:, kc, 0:128], rhs=cqnT[:, kc, 0:n], start=(kc == 0), stop=(kc == 3))
                    return ins
                P.op("pe", mmq, reads=[wk, "cqnT"], writes=["psA4"])
                P.op("act", lambda e: e.mul(out=QnT[:, 0:n], in_=psA[4][:, 0:n], mul=SCALE), reads=["psA4"], writes=[f"QnT{hs}"])

                def mmqr(e):
                    for kc in range(4):
                        ins = e.matmul(psA[5][0:64, 0:n], lhsT=wq[:, kc, 128:192], rhs=cqnT[:, kc, 0:n], start=(kc == 0), stop=(kc == 3))
                    return ins
                P.op("pe", mmqr, reads=[wk, "cqnT"], writes=["psA5"])
                P.op("dve", lambda e: e.tensor_tensor(out=t1[0:64, 0:n], in0=psA[5][0:64, 0:n], in1=cs_sb[:, 0, 0:n], op=ALU.mult), reads=["psA5", "cs_sb"], writes=["t1"])

                def mmqr2(e):
                    for kc in range(4):
                        ins = e.matmul(psA[5][0:64, 0:n], lhsT=wqrot[:, kc, :], rhs=cqnT[:, kc, 0:n], start=(kc == 0), stop=(kc == 3))
                    return ins
                P.op("pe", mmqr2, reads=["wqrot", "cqnT"], writes=["psA5"])
                P.op("dve", lambda e: e.tensor_tensor(out=t2[0:64, 0:n], in0=psA[5][0:64, 0:n], in1=cs_sb[:, 1, 0:n], op=ALU.mult), reads=["psA5", "cs_sb"], writes=["t2"])
                P.op("dve", lambda e: e.tensor_tensor(out=t1[0:64, 0:n], in0=t1[0:64, 0:n], in1=t2[0:64, 0:n], op=ALU.add), reads=["t1", "t2"], writes=["t1"])
                P.op("dve", lambda e: e.tensor_scalar(out=qrT[:, 0:n], in0=t1[0:64, 0:n], scalar1=SCALE, scalar2=None, op0=ALU.mult), reads=["t1"], writes=[f"qrT{hs}"])

            def attn(h):
                hs = h % 2
                KnT, Vh, QnT, qrT = HB[hs]["KnT"], HB[hs]["Vh"], HB[hs]["QnT"], HB[hs]["qrT"]

                def c0_of(j):
                    return 0 if j < q0 else (j - q0) * 128

                def emit_S(j):
                    c0 = c0_of(j)
                    b = j % 2

                    def mms(e):
                        e.matmul(psA[b][:, c0:n], lhsT=KnT[:, j * 128:(j + 1) * 128], rhs=QnT[:, c0:n], start=True, stop=False)
                        return e.matmul(psA[b][:, c0:n], lhsT=krT[:, j * 128:(j + 1) * 128], rhs=qrT[:, c0:n], start=False, stop=True)
                    P.op("pe", mms, reads=[f"KnT{hs}", f"QnT{hs}", "krT", f"qrT{hs}"], writes=[f"psA{b}"])
                    pt = PT[j % 3]
                    bias = pbias if j < 16 else zbias
                    P.op("act", lambda e: e.activation(out=pt[:, c0:n], in_=psA[b][:, c0:n], func=AF.Exp, bias=bias[:, 0:1], scale=1.0),
                         reads=[f"psA{b}", "pbias", "zbias"], writes=[f"PT{j % 3}"])
                    if j >= q0:
                        P.op("dve", lambda e: e.tensor_tensor(out=pt[:, c0:c0 + 128], in0=pt[:, c0:c0 + 128], in1=trib[:, :], op=ALU.mult),
                             reads=[f"PT{j % 3}", "trib"], writes=[f"PT{j % 3}"])

                def emit_PV(j):
                    c0 = c0_of(j)
                    pt = PT[j % 3]
                    P.op("pe", lambda e: e.matmul(psA[2][:, c0:n], lhsT=Vh[:, j, :], rhs=pt[:, c0:n], start=(j == 0), stop=(j == nk - 1)),
                         reads=[f"Vh{hs}", f"PT{j % 3}"], writes=["psA2"])
                    P.op("pe", lambda e: e.matmul(psA[3][:, c0:n], lhsT=onesb[:, :], rhs=pt[:, c0:n], start=(j == 0), stop=(j == nk - 1)),
                         reads=["onesb", f"PT{j % 3}"], writes=["psA3"])

                emit_S(0)
                for j in range(nk):
                    if j + 1 < nk:
                        emit_S(j + 1)
                    emit_PV(j)
                P.op("dve", lambda e: e.tensor_scalar(out=rs[:, 0:n], in0=psA[3][:, 0:n], scalar1=1e-30, scalar2=None, op0=ALU.add), reads=["psA3"], writes=["rs"])
                P.op("dve", lambda e: e.reciprocal(out=rs[:, 0:n], in_=rs[:, 0:n]), reads=["rs"], writes=["rs"])
                P.op("dve", lambda e: e.tensor_tensor(out=YT[:, 8 + h, 0:n], in0=psA[2][:, 0:n], in1=rs[:, 0:n], op=ALU.mult), reads=["psA2", "rs"], writes=[f"YT{8 + h}"])

            gen(0)
            for h in range(8):
                if h + 1 < 8:
                    gen(h + 1)
                attn(h)
            cnt = 0
            for cb in range(4):
                s = W.get([lambda e, t, cb=cb: e.dma_start(out=t[:, :].rearrange("p (k n) -> p k n", n=512), in_=kcv(w_out)[:, :, cb * 512:(cb + 1) * 512])])
                wv = slots[s][:, :].rearrange("p (k n) -> p k n", n=512)
                wk = f"ws{s}"
                for ti in range(nt):
                    b = 4 + (cnt % 2)
                    cnt += 1

                    def mmo2(e, ti=ti, b=b, wv=wv):
                        for kc in range(16):
                            ins = e.matmul(psA[b][:, :], lhsT=YT[:, kc, ti * 128:(ti + 1) * 128], rhs=wv[:, kc, :], start=(kc == 0), stop=(kc == 15))
                        return ins
                    P.op("pe", mmo2, reads=[wk] + [f"YT{c}" for c in range(16)], writes=[f"psA{b}"])
                    xs_ = xacc[:, ti, cb * 512:(cb + 1) * 512]
                    P.op("dve", lambda e, b=b, xs_=xs_: e.tensor_tensor(out=xs_, in0=psA[b][:, :], in1=xs_, op=ALU.add), reads=[f"psA{b}", f"xacc{ti}"], writes=[f"xacc{ti}"])
            P.barrier()
            if stop == "x1":
                if not halo:
                    dump(blk * TB, 4)
                return

            AR.reset()
            hT = AR.get([128, 16, TB], BF16)
            aT = AR.get([128, NF, TB], BF16)
            sg = [AR.get([128, TB], F32) for _ in range(2)]
            norm_T(nt, G_FFN0, hT)
            for hf in range(2):
                expert(hT, aT, sg, nt, ffn_w_gate[:, hf * FE:(hf + 1) * FE], ffn_w_up[:, hf * FE:(hf + 1) * FE], ffn_w_down[hf * FE:(hf + 1) * FE, :], None)
            P.barrier()
            if stop == "x2":
                if not halo:
                    dump(blk * TB, 4)
                return

            AR.reset()
            hTp = AR.get([128, 16, 16 + TB], BF16)
            norm_T(nt, G_MIX1, hTp, col0=16)
            if halo:
                P.op("dve", lambda e: e.tensor_copy(out=h1halo[:, :, :], in_=hTp[:, :, 16 + 112:16 + 128]), reads=hT_keys(1), writes=["h1halo"])
                P.barrier()
                return
            pooledT = AR.get([128, 16, TB], BF16)
            sA = AR.get([128, 16 + TB], F32)
            sB = AR.get([128, 16 + TB], F32)
            ptmp = AR.get([128, 512], F32)
            P.op("dve", lambda e: e.tensor_copy(out=hTp[:, :, 0:16], in_=h1halo[:, :, :]), reads=["h1halo"], writes=["hThalo"])
            P.op("dve", lambda e: e.tensor_copy(out=h1halo[:, :, :], in_=hTp[:, :, n:n + 16]), reads=hT_keys(nt) + ["hThalo"], writes=["h1halo"])
            for g in range(4):
                wwin = 2 ** (g + 1)
                for kc in range(4):
                    ch = g * 4 + kc
                    src = hTp[:, ch, :]
                    sk = None
                    bufs = [sA, sB]
                    for li in range(g + 1):
                        sh = 2 ** li
                        lo = 2 * sh - 1
                        dst = bufs[li % 2]
                        dk = "sA" if li % 2 == 0 else "sB"
                        rd = (hT_keys(nt) + ["hThalo"]) if li == 0 else [sk]
                        P.op("dve", lambda e, src=src, dst=dst, lo=lo, sh=sh: e.tensor_tensor(out=dst[:, lo:16 + n], in0=src[:, lo:16 + n], in1=src[:, lo - sh:16 + n - sh], op=ALU.add),
                             reads=rd, writes=[dk])
                        src = dst
                        sk = dk
                    if blk == 0:
                        P.op("dve", lambda e, src=src, g=g: e.tensor_tensor(out=src[:, 16:32], in0=src[:, 16:32], in1=pfix[:, g * 16:(g + 1) * 16], op=ALU.mult),
                             reads=[sk, "pfix"], writes=[sk])
                    P.op("dve", lambda e, src=src, ch=ch, wwin=wwin: e.scalar_tensor_tensor(out=pooledT[:, ch, 0:n], in0=src[:, 16:16 + n], scalar=1.0 / wwin, in1=hTp[:, ch, 16:16 + n], op0=ALU.mult, op1=ALU.subtract),
                         reads=[sk] + hT_keys(nt), writes=[f"pT{ch}"])
                s = W.get([lambda e, t, g=g: e.dma_start(out=t[:, 0:2048].rearrange("p (k n) -> p k n", n=512), in_=pool_w[g].rearrange("(kc p) n -> p kc n", p=128))])
                wv = slots[s][:, 0:2048].rearrange("p (k n) -> p k n", n=512)
                wk = f"ws{s}"
                P.dma("sp", "psc", [lambda e, g=g: e.dma_start(out=psc[:, :], in_=pool_scale[g * 512:(g + 1) * 512].rearrange("(o n) -> o n", o=1).to_broadcast([128, 512]))], writes=["psc"])
                for ti in range(nt):
                    b = 4 + (ti % 2)

                    def mmp(e, ti=ti, b=b, wv=wv, g=g):
                        for kc in range(4):
                            ins = e.matmul(psA[b][:, :], lhsT=pooledT[:, g * 4 + kc, ti * 128:(ti + 1) * 128], rhs=wv[:, kc, :], start=(kc == 0), stop=(kc == 3))
                        return ins
                    P.op("pe", mmp, reads=[wk] + [f"pT{g * 4 + kc}" for kc in range(4)], writes=[f"psA{b}"])
                    xs_ = xacc[:, ti, g * 512:(g + 1) * 512]
                    P.op("dve", lambda e, b=b: e.tensor_tensor(out=ptmp[:, :], in0=psA[b][:, :], in1=psc[:, :], op=ALU.mult), reads=[f"psA{b}", "psc"], writes=["ptmp"])
                    P.op("dve", lambda e, xs_=xs_: e.tensor_tensor(out=xs_, in0=ptmp[:, :], in1=xs_, op=ALU.add), reads=["ptmp", f"xacc{ti}"], writes=[f"xacc{ti}"])
            P.barrier()
            if stop == "x3":
                dump(blk * TB, 4)
                return

            AR.reset()
            hT = AR.get([128, 16, TB], BF16)
            aT = AR.get([128, NF, TB], BF16)
            sg = [AR.get([128, TB], F32) for _ in range(2)]
            norm_T(nt, G_FFN1, hT)
            s = W.get([lambda e, t: e.dma_start(out=t[:, 0:128].rearrange("p (k n) -> p k n", n=8), in_=router_w.rearrange("(kc p) n -> p kc n", p=128))])
            wv = slots[s][:, 0:128].rearrange("p (k n) -> p k n", n=8)
            wk = f"ws{s}"
            for ti in range(nt):
                b = 4 + (ti % 2)
                r0 = ti * 16

                def mmr_(e, ti=ti, b=b, wv=wv):
                    for kc in range(16):
                        ins = e.matmul(psA[b][:, 0:8], lhsT=hT[:, kc, ti * 128:(ti + 1) * 128], rhs=wv[:, kc, :], start=(kc == 0), stop=(kc == 15))
                    return ins
                P.op("pe", mmr_, reads=[wk] + hT_keys(nt), writes=[f"psA{b}"])
                rk_ = f"rt{ti}"
                P.op("act", lambda e, b=b, r0=r0: e.activation(out=rt[:, r0:r0 + 8], in_=psA[b][:, 0:8], func=AF.Copy), reads=[f"psA{b}"], writes=[rk_])
                P.op("dve", lambda e, r0=r0: e.max(out=rt[:, r0 + 8:r0 + 16], in_=rt[:, r0:r0 + 8]), reads=[rk_], writes=[rk_])
                P.op("dve", lambda e, r0=r0, ti=ti: e.tensor_scalar(out=gates[:, ti, :], in0=rt[:, r0:r0 + 8], scalar1=rt[:, r0 + 9:r0 + 10], scalar2=0.0, op0=ALU.subtract, op1=ALU.is_ge),
                     reads=[rk_], writes=["gates"])
                P.op("dve", lambda e, r0=r0: e.tensor_scalar(out=rt[:, r0 + 10:r0 + 11], in0=rt[:, r0 + 8:r0 + 9], scalar1=-1.0, scalar2=None, op0=ALU.mult), reads=[rk_], writes=[rk_])
                P.op("act", lambda e, r0=r0: e.activation(out=rt[:, r0:r0 + 8], in_=rt[:, r0:r0 + 8], func=AF.Exp, bias=rt[:, r0 + 10:r0 + 11], scale=1.0), reads=[rk_], writes=[rk_])
                P.op("dve", lambda e, r0=r0, ti=ti: e.tensor_tensor(out=rt[:, r0:r0 + 8], in0=rt[:, r0:r0 + 8], in1=gates[:, ti, :], op=ALU.mult), reads=[rk_, "gates"], writes=[rk_])
                P.op("dve", lambda e, r0=r0: e.tensor_reduce(out=rt[:, r0 + 11:r0 + 12], in_=rt[:, r0:r0 + 8], axis=AX.X, op=ALU.add), reads=[rk_], writes=[rk_])
                P.op("dve", lambda e, r0=r0: e.reciprocal(out=rt[:, r0 + 12:r0 + 13], in_=rt[:, r0 + 11:r0 + 12]), reads=[rk_], writes=[rk_])
                P.op("dve", lambda e, r0=r0, ti=ti: e.tensor_scalar(out=gates[:, ti, :], in0=rt[:, r0:r0 + 8], scalar1=rt[:, r0 + 12:r0 + 13], scalar2=None, op0=ALU.mult), reads=[rk_], writes=["gates"])
            for ex in range(8):
                expert(hT, aT, sg, nt, moe_w_gate[ex], moe_w_up[ex], moe_w_down[ex], ex)
            P.barrier()
            if stop == "x4":
                dump(blk * TB, 4)
                return

            for ti in range(nt):
                rstd_of(ti)
                c = 8 * ti
                P.op("dve", lambda e, ti=ti, c=c: e.scalar_tensor_tensor(out=ot[:, :], in0=xacc[:, ti, :], scalar=st[:, c + 7:c + 8], in1=fgbc[:, :], op0=ALU.mult, op1=ALU.mult),
                     reads=[f"xacc{ti}", f"st{ti}", "fgbc"], writes=["ot"])
                kv = P.dma("sp", "ot", [lambda e, ti=ti, blk=blk: e.dma_start(out=out[blk * TB + ti * 128:blk * TB + (ti + 1) * 128, :], in_=ot[:, :])], reads=["ot"])
                final_kvs.append(kv)

        for blk in range(-1, NBLK):
            do_block(blk)
        P.wait_all("sp", [kv for kv in final_kvs if kv is not None])

    Pd = Prog(nc, dry=True)
    Wd = WStream(Pd, slots)
    program(Pd, Wd)
    P = Prog(nc)
    W = WStream(P, slots, sched=Wd.sched)
    program(P, W)
    P.emit()
    return nc


def _host_tables(half):
    inv_freq = (10000.0 ** (-np.arange(0, 64, 2, dtype=np.float32) / np.float32(64))).astype(np.float32)
    cf = np.zeros((64, 4), np.float32)
    cf[:, 0] = np.concatenate([inv_freq, inv_freq])
    cf[:, 1] = float(half * TOK)
    cf[:, 2] = math.pi
    cf[:, 3] = math.pi / 2
    pfix = np.ones((4, 16), np.float32)
    if half == 0:
        for g, w in enumerate((2, 4, 8, 16)):
            for t in range(16):
                pfix[g, t] = w / min(t + 1, w)
    pfix = np.ascontiguousarray(np.broadcast_to(pfix.reshape(1, 64), (128, 64))).astype(np.float32)
    pbias = np.full((128, 1), 0.0 if half == 1 else -30000.0, np.float32)
    return cf, pfix, pbias


_NC_CACHE = {}


def kernel(**inputs):
    stop = os.environ.get("KSTOP", "y")
    x = np.asarray(inputs["x"], dtype=np.float32)
    shared = {}
    for k, v in inputs.items():
        if k == "x":
            continue
        a = np.asarray(v, dtype=np.float32)
        if k != "final_norm":
            a = a[0]
        shared[k] = np.ascontiguousarray(a)
    shared["ident"] = np.eye(128, dtype=np.float32)
    shared["iota"] = np.ascontiguousarray(np.broadcast_to(np.arange(TB, dtype=np.float32), (64, TB)))
    shared["tri"] = np.triu(np.ones((128, 128), np.float32))
    in_maps = []
    for c in range(8):
        b, half = c // 2, c % 2
        cf, pfix, pbias = _host_tables(half)
        m = dict(shared)
        m["xo"] = np.ascontiguousarray(x[b, half * TOK:(half + 1) * TOK])
        m["xp"] = np.ascontiguousarray(x[b, 0:TOK]) if half == 1 else np.zeros((TOK, D), np.float32)
        m["cf"] = cf
        m["pfix"] = pfix
        m["pbias"] = pbias
        in_maps.append(m)
    if stop not in _NC_CACHE:
        _NC_CACHE[stop] = build(stop)
    nc = _NC_CACHE[stop]
    res = run_bass_kernel_spmd(nc, in_maps, core_ids=list(range(8)))
    y = np.empty((4, 4096, D), np.float32)
    for c in range(8):
        b, half = c // 2, c % 2
        y[b, half * TOK:(half + 1) * TOK] = res.results[c]["out"]
    return y
```
